# Optimizing a Trainium2 kernel written in Bass

```python
import jax, jax.numpy as jnp
from jax import lax
import numpy as np

D_MODEL = 4096
BATCH = 8
SEQ = 2048
DEPTH = 2

CHUNK = 64
HEAD_DIM = 128
A_HEADS = (3 * D_MODEL // 8) // HEAD_DIM
A_WIDTH = A_HEADS * HEAD_DIM
B_HEADS = (3 * D_MODEL // 8) // HEAD_DIM
B_WIDTH = B_HEADS * HEAD_DIM
C_WIDTH = D_MODEL - A_WIDTH - B_WIDTH
POOL_WINDOWS = (2, 4, 8, 16)
POOL_GROUP = C_WIDTH // len(POOL_WINDOWS)
IN_COLS = 4 * A_WIDTH + 3 * B_WIDTH + C_WIDTH
BAND_CHUNKS = 9
MAX_REL_DIST = 128
D_FF = 256 * ((8 * D_MODEL // 3 + 255) // 256)
N_EXPERTS = 8
TOP_K = 2
D_EXPERT = 5 * D_MODEL // 4
N_DENSE = (DEPTH + 1) // 2
N_MOE = DEPTH // 2
N_MOD = 6
EPS = 1e-6
F_MIN = 1e-6
MASK_VALUE = -1e9

kernel_name = "hybrid_hgrn2_chunkattn_pool_moe_trunk"


def rms_norm(x, w):
    xf = x.astype(jnp.float32)
    y = xf * lax.rsqrt(jnp.mean(xf * xf, axis=-1, keepdims=True) + EPS)
    return (y * w.astype(jnp.float32)).astype(x.dtype)


def hgrn2_mixer(q, fz, inp, g, lb, norm_w):
    bsz, slen = q.shape[:2]
    nc = slen // CHUNK
    f32 = jnp.float32
    zf = fz.astype(f32)
    lbf = lb.astype(f32)
    f_gate = lbf + (1.0 - lbf) * jax.nn.sigmoid(zf)
    log_f = jnp.log(jnp.maximum(f_gate, F_MIN))
    key = (1.0 - lbf) * jax.nn.sigmoid(-zf)

    def heads(t):
        return t.reshape(bsz, nc, CHUNK, A_HEADS, HEAD_DIM).transpose(1, 0, 3, 2, 4)

    qh = heads(q.astype(f32))
    kh = heads(key)
    vh = heads(inp.astype(f32))
    bh = jnp.cumsum(heads(log_f), axis=3)
    causal = jnp.tril(jnp.ones((CHUNK, CHUNK), dtype=bool))

    def step(state, xs):
        qc, kc, vc, bc = xs
        inter = jnp.einsum('bhtk,bhkv->bhtv', qc * jnp.exp(bc), state)
        diff = bc[:, :, :, None, :] - bc[:, :, None, :, :]
        decay = jnp.where(causal[:, :, None], jnp.exp(jnp.minimum(diff, 0.0)), 0.0)
        scores = jnp.einsum('bhtk,bhsk,bhtsk->bhts', qc, kc, decay)
        out = inter + jnp.einsum('bhts,bhsv->bhtv', scores, vc)
        b_last = bc[:, :, -1:, :]
        state = (jnp.exp(b_last[:, :, 0, :])[..., None] * state
                 + jnp.einsum('bhsk,bhsv->bhkv', kc * jnp.exp(b_last - bc), vc))
        return state, out

    s0 = jnp.zeros((bsz, A_HEADS, HEAD_DIM, HEAD_DIM), f32)
    _, o = lax.scan(step, s0, (qh, kh, vh, bh))
    o = o.transpose(1, 0, 3, 2, 4).reshape(bsz, slen, A_HEADS, HEAD_DIM)
    gh = g.astype(f32).reshape(bsz, slen, A_HEADS, HEAD_DIM)
    o = rms_norm(o, norm_w) * jax.nn.silu(gh)
    return o.reshape(bsz, slen, A_WIDTH).astype(q.dtype)


def chunk_band_attention(q, k, v, qn_w, kn_w, rel_table):
    bsz, slen = q.shape[:2]
    nc = slen // CHUNK
    qh = rms_norm(q.reshape(bsz, slen, B_HEADS, HEAD_DIM), qn_w).transpose(0, 2, 1, 3)
    kh = rms_norm(k.reshape(bsz, slen, B_HEADS, HEAD_DIM), kn_w).transpose(0, 2, 1, 3)
    vh = v.reshape(bsz, slen, B_HEADS, HEAD_DIM).transpose(0, 2, 1, 3)
    pad = (BAND_CHUNKS - 1) * CHUNK
    band = BAND_CHUNKS * CHUNK
    k_pad = jnp.pad(kh, ((0, 0), (0, 0), (pad, 0), (0, 0)))
    v_pad = jnp.pad(vh, ((0, 0), (0, 0), (pad, 0), (0, 0)))
    rel = pad + jnp.arange(CHUNK)[:, None] - jnp.arange(band)[None, :]
    bias = rel_table.astype(jnp.float32)[:, jnp.clip(rel, -MAX_REL_DIST, MAX_REL_DIST) + MAX_REL_DIST]
    key_offset = jnp.arange(band) - pad
    scale = HEAD_DIM ** -0.5

    def one_chunk(ci):
        start = ci * CHUNK
        qc = lax.dynamic_slice_in_dim(qh, start, CHUNK, axis=2)
        kc = lax.dynamic_slice_in_dim(k_pad, start, band, axis=2)
        vc = lax.dynamic_slice_in_dim(v_pad, start, band, axis=2)
        s = jnp.einsum('bhqd,bhkd->bhqk', qc, kc).astype(jnp.float32) * scale + bias
        s = jnp.where((start + key_offset) >= 0, s, MASK_VALUE)
        p = jax.nn.softmax(s, axis=-1).astype(vc.dtype)
        return jnp.einsum('bhqk,bhkd->bhqd', p, vc)

    o = lax.map(one_chunk, jnp.arange(nc))
    return o.transpose(1, 0, 3, 2, 4).reshape(bsz, slen, B_WIDTH)


def pool_mixer(p, w_pool, pool_scale):
    slen = p.shape[1]
    pf = p.astype(jnp.float32)
    cs = jnp.pad(jnp.cumsum(pf, axis=1), ((0, 0), (1, 0), (0, 0)))
    pos = jnp.arange(slen) + 1
    outs = []
    for gi, w in enumerate(POOL_WINDOWS):
        sl = slice(gi * POOL_GROUP, (gi + 1) * POOL_GROUP)
        csg = cs[:, :, sl]
        lower = jnp.pad(csg[:, :slen + 1 - w], ((0, 0), (w - 1, 0), (0, 0)))
        cnt = jnp.minimum(pos, w).astype(jnp.float32)[None, :, None]
        mixed = (csg[:, 1:] - lower) / cnt - pf[:, :, sl]
        outs.append(jnp.einsum('bsc,cd->bsd', mixed.astype(p.dtype), w_pool[gi]))
    return jnp.concatenate(outs, axis=-1) * pool_scale


def swiglu(h, w_gate, w_up, w_down):
    return jnp.einsum('bsf,fd->bsd', jax.nn.silu(jnp.einsum('bsd,df->bsf', h, w_gate)) * jnp.einsum('bsd,df->bsf', h, w_up), w_down)


def moe_ffn(h, w_router, b_router, w_gate, w_up, w_down):
    bsz, slen, d = h.shape
    hf = h.reshape(bsz * slen, d)
    logits = jnp.einsum('td,de->te', hf, w_router).astype(jnp.float32) + b_router.astype(jnp.float32)
    top_v, top_i = lax.top_k(logits, TOP_K)
    top_w = jax.nn.softmax(top_v, axis=-1)
    combine = jnp.sum(jax.nn.one_hot(top_i, N_EXPERTS, dtype=jnp.float32) * top_w[..., None], axis=1)
    combine = combine.astype(h.dtype)
    out = jnp.zeros_like(hf)
    for e in range(N_EXPERTS):
        he = jax.nn.silu(hf @ w_gate[e]) * (hf @ w_up[e])
        out = out + combine[:, e:e + 1] * (he @ w_down[e])
    return out.reshape(bsz, slen, d)


def setup_inputs(seed: int = 0) -> dict:
    key = jax.random.key(seed)
    ks = jax.random.split(key, 24)
    f32 = jnp.float32
    nrm = lambda k, shape, s: jax.random.normal(k, shape, f32) * s
    D = D_MODEL
    return {
        "x": nrm(ks[0], (BATCH, SEQ, D), 1.0),
        "c": nrm(ks[1], (BATCH, D), 1.0),
        "w_ada": nrm(ks[2], (D, N_MOD * D), 0.5 * D ** -0.5),
        "b_ada": nrm(ks[3], (N_MOD * D,), 0.02),
        "ada_table": nrm(ks[4], (DEPTH, N_MOD, D), 0.1),
        "norm_mix_w": 1.0 + nrm(ks[5], (DEPTH, D), 0.05),
        "w_in": nrm(ks[6], (DEPTH, D, IN_COLS), D ** -0.5),
        "lb_logits": nrm(ks[7], (DEPTH, A_WIDTH), 0.1),
        "hgrn_norm_w": 1.0 + nrm(ks[8], (DEPTH, HEAD_DIM), 0.05),
        "q_norm_w": 1.0 + nrm(ks[9], (DEPTH, HEAD_DIM), 0.05),
        "k_norm_w": 1.0 + nrm(ks[10], (DEPTH, HEAD_DIM), 0.05),
        "rel_bias": nrm(ks[11], (B_HEADS, 2 * MAX_REL_DIST + 1), 0.5),
        "w_pool": nrm(ks[12], (DEPTH, len(POOL_WINDOWS), POOL_GROUP, POOL_GROUP), POOL_GROUP ** -0.5),
        "pool_scale": 1.0 + nrm(ks[13], (DEPTH, C_WIDTH), 0.1),
        "w_o": nrm(ks[14], (DEPTH, D, D), D ** -0.5),
        "norm_ffn_w": 1.0 + nrm(ks[15], (DEPTH, D), 0.05),
        "ffn_w_gate": nrm(ks[16], (N_DENSE, D, D_FF), D ** -0.5),
        "ffn_w_up": nrm(ks[17], (N_DENSE, D, D_FF), D ** -0.5),
        "ffn_w_down": nrm(ks[18], (N_DENSE, D_FF, D), D_FF ** -0.5),
        "moe_w_router": nrm(ks[19], (N_MOE, D, N_EXPERTS), D ** -0.5),
        "moe_b_router": nrm(ks[20], (N_MOE, N_EXPERTS), 0.01),
        "moe_w_gate": nrm(ks[21], (N_MOE, N_EXPERTS, D, D_EXPERT), D ** -0.5),
        "moe_w_up": nrm(ks[22], (N_MOE, N_EXPERTS, D, D_EXPERT), D ** -0.5),
        "moe_w_down": nrm(ks[23], (N_MOE, N_EXPERTS, D_EXPERT, D), D_EXPERT ** -0.5),
    }


def reference(x, c, w_ada, b_ada, ada_table, norm_mix_w, w_in, lb_logits, hgrn_norm_w,
              q_norm_w, k_norm_w, rel_bias, w_pool, pool_scale, w_o, norm_ffn_w,
              ffn_w_gate, ffn_w_up, ffn_w_down, moe_w_router, moe_b_router,
              moe_w_gate, moe_w_up, moe_w_down):
    bsz = x.shape[0]
    mod = (jnp.einsum('bd,de->be', jax.nn.silu(c), w_ada) + b_ada).reshape(bsz, N_MOD, D_MODEL)
    lb_p = jax.nn.softmax(lb_logits.astype(jnp.float32), axis=0)
    lb_all = jnp.cumsum(lb_p, axis=0) - lb_p[0:1]
    a0, a1, a2, a3 = A_WIDTH, 2 * A_WIDTH, 3 * A_WIDTH, 4 * A_WIDTH
    b1, b2, b3 = a3 + B_WIDTH, a3 + 2 * B_WIDTH, a3 + 3 * B_WIDTH
    for l in range(DEPTH):
        m = (mod + ada_table[l][None]).astype(x.dtype)
        shift1, scale1, gate1 = m[:, 0, None], m[:, 1, None], m[:, 2, None]
        shift2, scale2, gate2 = m[:, 3, None], m[:, 4, None], m[:, 5, None]
        h = rms_norm(x, norm_mix_w[l]) * (1.0 + scale1) + shift1
        proj = jnp.einsum('bsd,de->bse', h, w_in[l])
        y_a = hgrn2_mixer(proj[..., :a0], proj[..., a0:a1], proj[..., a1:a2], proj[..., a2:a3],
                          lb_all[l], hgrn_norm_w[l])
        y_b = chunk_band_attention(proj[..., a3:b1], proj[..., b1:b2], proj[..., b2:b3],
                                   q_norm_w[l], k_norm_w[l], rel_bias)
        y_c = pool_mixer(proj[..., b3:], w_pool[l], pool_scale[l])
        y = jnp.einsum('bse,ed->bsd', jnp.concatenate([y_a, y_b, y_c], axis=-1), w_o[l])
        x = x + gate1 * y
        h = rms_norm(x, norm_ffn_w[l]) * (1.0 + scale2) + shift2
        if l % 2 == 0:
            f = swiglu(h, ffn_w_gate[l // 2], ffn_w_up[l // 2], ffn_w_down[l // 2])
        else:
            j = l // 2
            f = moe_ffn(h, moe_w_router[j], moe_b_router[j], moe_w_gate[j], moe_w_up[j], moe_w_down[j])
        x = x + gate2 * f
    return x
```

```python
import numpy as np
from contextlib import ExitStack
import concourse.bass as bass
import concourse.mybir as mybir
from concourse.bass_utils import run_bass_kernel_spmd

F32 = mybir.dt.float32
BF16 = mybir.dt.bfloat16
AF = mybir.ActivationFunctionType
ALU = mybir.AluOpType
AX = mybir.AxisListType

EPS = 1e-6
F_MIN = 1e-6
NEG = -30000.0
CH = 64


class Cfg:
    def __init__(self, D=4096, S=2048, n_cores=8):
        self.D = D
        self.T = S
        self.KD = D // 128
        self.AH = (3 * D // 8) // 128
        self.AW = self.AH * 128
        self.BH = self.AH
        self.BW = self.AW
        self.CW = D - self.AW - self.BW
        self.PG = self.CW // 4
        self.PGT = self.PG // 128
        self.IN_COLS = 4 * self.AW + 3 * self.BW + self.CW
        self.DFF = 256 * ((8 * D // 3 + 255) // 256)
        self.NE = 8
        self.DEXP = 5 * D // 4
        self.NMOD = 6
        self.n_cores = n_cores
        self.NH = S // 1024
        self.NTT = S // 128
        self.NCH = S // CH
        assert S % 1024 == 0 and self.PG % 128 == 0


class DSlot:
    _n = 0

    def __init__(self, nc, name):
        DSlot._n += 1
        self.h = nc.alloc_semaphore("%s_%d" % (name, DSlot._n))
        self.v = 0


class TR:
    def __init__(self, nc):
        self.nc = nc
        self.eng = {"pe": nc.tensor, "act": nc.scalar, "dve": nc.vector, "pool": nc.gpsimd, "sp": nc.sync}
        self.sem = {k: nc.alloc_semaphore("cnt_" + k) for k in self.eng}
        self.cnt = {k: 0 for k in self.eng}
        self.lastw = {}
        self.reads = {}
        self.seen = {k: {} for k in self.eng}
        self.slots = []

    def slot(self, name):
        if not hasattr(self, "_slotcache"):
            self._slotcache = {}
        if name in self._slotcache:
            return self._slotcache[name]
        s = DSlot(self.nc, name)
        self.slots.append(s)
        self._slotcache[name] = s
        return s

    def _deps(self, reads, writes):
        deps = []
        for b in reads:
            t = self.lastw.get(b)
            if t is not None:
                deps.append(t)
        for b in writes:
            t = self.lastw.get(b)
            if t is not None:
                deps.append(t)
            r = self.reads.get(b)
            if r:
                deps.extend(r.values())
        return deps

    def _wait(self, e, deps):
        for (s, v, owner) in deps:
            if owner == e and e in ("pe", "sp"):
                continue
            if v <= 0:
                continue
            k = id(s)
            if self.seen[e].get(k, 0) >= v:
                continue
            self.eng[e].wait_ge(s, v)
            self.seen[e][k] = v

    def _record(self, tok, reads, writes):
        for b in writes:
            self.lastw[b] = tok
            self.reads[b] = {}
        for b in reads:
            self.reads.setdefault(b, {})[id(tok[0])] = tok

    def op(self, e, fn, reads=(), writes=(), inc=True):
        self._wait(e, self._deps(reads, writes))
        ins = fn(self.eng[e])
        tok = (self.sem[e], self.cnt[e] + 1, e)
        if inc:
            self.cnt[e] += 1
            ins.then_inc(self.sem[e], 1)
        self._record(tok, reads, writes)
        return ins

    def dma(self, e, slot, fn, reads=(), writes=()):
        deps = self._deps(reads, writes)
        if slot.v > 0:
            deps.append((slot.h, slot.v, None))
        self._wait(e, deps)
        ins = fn(self.eng[e])
        slot.v += 16
        ins.then_inc(slot.h, 16)
        self._record((slot.h, slot.v, None), reads, writes)
        return ins

    def barrier(self):
        toks = [(self.sem[k], self.cnt[k], k) for k in self.eng if self.cnt[k] > 0]
        toks += [(s.h, s.v, None) for s in self.slots if s.v > 0]
        for e in self.eng:
            self._wait(e, [t for t in toks if t[2] != e])
        self.lastw.clear()
        self.reads.clear()


def build_program(cfg, stop_after=None, debug=False, groups="ABC"):
    D, T, KD = cfg.D, cfg.T, cfg.KD
    AH, AW, BH, BW, CW = cfg.AH, cfg.AW, cfg.BH, cfg.BW, cfg.CW
    NH, NTT, NCH = cfg.NH, cfg.NTT, cfg.NCH
    NE, DEXP, DFF = cfg.NE, cfg.DEXP, cfg.DFF
    nc = bass.Bass("TRN2", target_bir_lowering=False)

    def din(name, shape):
        return nc.dram_tensor(name, list(shape), F32, kind="ExternalInput").ap()

    x_d = din("x", [T, D])
    c_d = din("c", [KD, 128])
    w_ada = din("w_ada", [D, 6 * D])
    b_ada = din("b_ada", [6 * KD, 128])
    ada_table = din("ada_table", [2, 6 * KD, 128])
    norm_mix_w = din("norm_mix_w", [2, KD, 128])
    w_in = din("w_in", [2, D, cfg.IN_COLS])
    lb_logits = din("lb_logits", [2, AH, 128])
    hgrn_norm_w = din("hgrn_norm_w", [2, 1, 128])
    q_norm_w = din("q_norm_w", [2, 1, 128])
    k_norm_w = din("k_norm_w", [2, 1, 128])
    rel_bias = din("rel_bias", [BH, 257])
    w_pool = din("w_pool", [2, 4, cfg.PG, cfg.PG])
    pool_scale = din("pool_scale", [2, CW // 128, 128])
    w_o = din("w_o", [2, D, D])
    norm_ffn_w = din("norm_ffn_w", [2, KD, 128])
    ffn_w_gate = din("ffn_w_gate", [1, D, DFF])
    ffn_w_up = din("ffn_w_up", [1, D, DFF])
    ffn_w_down = din("ffn_w_down", [1, DFF, D])
    moe_w_router = din("moe_w_router", [1, D, NE])
    moe_b_router = din("moe_b_router", [1, NE])
    moe_w_gate = din("moe_w_gate", [1, NE, D, DEXP])
    moe_w_up = din("moe_w_up", [1, NE, D, DEXP])
    moe_w_down = din("moe_w_down", [1, NE, DEXP, D])
    NCONST = 128 + 128 + T + 16
    consts_d = din("consts", [128, NCONST])
    out_d = nc.dram_tensor("out", [T, D], F32, kind="ExternalOutput").ap()

    def dscr(name, shape, dt):
        return nc.dram_tensor(name, list(shape), dt, kind=("ExternalOutput" if debug else "Internal")).ap()

    xT = dscr("xT", [D, T], F32)
    projT = dscr("projT", [cfg.IN_COLS, T], F32)
    yT_d = dscr("yT", [D, T], BF16)
    FTOT = max(DFF, NE * DEXP)
    actT = dscr("actT", [FTOT, T], BF16)
    combT = dscr("combT", [NE, 128, T], F32)
    Zd = dscr("Zd", [BH, 128 * 768], F32)

    tr = TR(nc)
    ES = ExitStack()

    sbn = {"n": 0}

    def sb(es, name, shape, dt):
        sbn["n"] += 1
        return es.enter_context(nc.sbuf_tensor("%s_%d" % (name, sbn["n"]), list(shape), dt))

    PSA = ES.enter_context(nc.psum_tensor("PSA", [128, 4096], F32))

    def bank(b, n=512, off=0):
        return PSA[:, b * 512 + off: b * 512 + off + n]

    def unitps(u):
        return PSA[:, u * 1024:(u + 1) * 1024]

    PSB = PSA[:, :].bitcast(BF16)

    cst = sb(ES, "cst", [128, NCONST], F32)
    ident = cst[:, 0:128]
    cmask = cst[:, 128:256]
    rmask = cst[:, 256:256 + T]
    invc = cst[:, 256 + T:256 + T + 16]
    identb = sb(ES, "identb", [128, 128], BF16)
    onesb = sb(ES, "onesb", [128, 128], BF16)
    ones32 = sb(ES, "ones32", [128, 128], F32)
    epsc = sb(ES, "epsc", [128, 1], F32)

    rows = []
    rowpos = {}

    def addrows(key, ap, n):
        start = len(rows)
        if (start % 128) + n > 128:
            start = (start // 128 + 1) * 128
            while len(rows) < start:
                rows.append(None)
        rowpos[key] = start
        for i in range(n):
            rows.append((key, i))
        rowpos[(key, "ap")] = ap

    addrows("c", c_d, KD)
    for m in range(6):
        addrows(("b_ada", m), b_ada[m * KD:(m + 1) * KD, :], KD)
    for l in range(2):
        for m in range(6):
            addrows(("ada", l, m), ada_table[l, m * KD:(m + 1) * KD, :], KD)
        addrows(("nmw", l), norm_mix_w[l], KD)
        addrows(("nfw", l), norm_ffn_w[l], KD)
        addrows(("lb", l), lb_logits[l], AH)
        addrows(("psc", l), pool_scale[l], CW // 128)
        addrows(("hnw", l), hgrn_norm_w[l], 1)
        addrows(("qnw", l), q_norm_w[l], 1)
        addrows(("knw", l), k_norm_w[l], 1)
    NRT = (len(rows) + 127) // 128
    PC = sb(ES, "PC", [128, NRT * 128], F32)

    def pcol(key, i=0, n=1):
        r = rowpos[key] + i
        return PC[:, r:r + n]

    MODW = 6 * KD
    Ml = [sb(ES, "Ml%d" % l, [128, MODW], F32) for l in range(2)]
    S1 = [sb(ES, "S1_%d" % l, [128, KD], F32) for l in range(2)]
    S2 = [sb(ES, "S2_%d" % l, [128, KD], F32) for l in range(2)]
    lbv = [sb(ES, "lbv%d" % l, [128, AH], F32) for l in range(2)]
    oml = [sb(ES, "oml%d" % l, [128, AH], F32) for l in range(2)]
    fml = [sb(ES, "fml%d" % l, [128, AH], F32) for l in range(2)]
    wqs = [sb(ES, "wqs%d" % l, [128, 1], F32) for l in range(2)]
    brt = sb(ES, "brt", [NE, 1], F32)

    NW = 4
    PF = 2
    WR = [sb(ES, "WR%d" % i, [128, 32, 128], BF16) for i in range(NW)]
    WRs = [tr.slot("wr%d" % i) for i in range(NW)]
    wstate = {"n": 0}

    def wload(pieces):
        i = wstate["n"] % NW
        wstate["n"] += 1
        for (kc0, nkc, ap) in pieces:
            src = ap.rearrange("(kc p) n -> p kc n", p=128)
            tr.dma("pool", WRs[i], lambda e, src=src, i=i, kc0=kc0, nkc=nkc: e.dma_start(out=WR[i][:, kc0:kc0 + nkc, :], in_=src),
                   writes=[("WR", i)])
        return i

    ustate = {"n": 0}

    def gemm(AT, atkey, tiles, evac, pair=False):
        nt = len(tiles)
        loaded = {}
        for i in range(min(PF, nt)):
            loaded[i] = wload(tiles[i]["pieces"])
        step = 2 if pair else 1
        for j0 in range(0, nt, step):
            for hf in range(NH):
                pss = []
                for j in range(j0, j0 + step):
                    if hf == 0 and j + PF < nt:
                        loaded[j + PF] = wload(tiles[j + PF]["pieces"])
                    wi = loaded[j]
                    KC = tiles[j]["KC"]
                    u = ustate["n"] % 4
                    ustate["n"] += 1
                    pskey = [("ps", 2 * u), ("ps", 2 * u + 1)]
                    for kc in range(KC):
                        for s in range(2):
                            last = (kc == KC - 1 and s == 1)
                            tr.op("pe", lambda e, u=u, s=s, wi=wi, kc=kc, hf=hf, KC=KC: e.matmul(
                                PSA[:, u * 1024 + s * 512: u * 1024 + (s + 1) * 512],
                                WR[wi][:, kc, :], AT[:, kc, hf * 1024 + s * 512: hf * 1024 + (s + 1) * 512],
                                start=(kc == 0), stop=(kc == KC - 1)),
                                reads=[("WR", wi), atkey], writes=[pskey[s]], inc=last)
                    pss.append((unitps(u), pskey))
                evac([tiles[j]["info"] for j in range(j0, j0 + step)], hf, pss)

    with ExitStack() as es:
        s_c = tr.slot("ld_c")
        tr.dma("sp", s_c, lambda e: e.dma_start(out=cst[:], in_=consts_d[:, :]), writes=["cst"])
        R = sb(es, "R", [128, NRT, 128], F32)
        tr.op("pool", lambda e: e.memset(R[:], 0.0), writes=["R"])
        tr.op("pool", lambda e: e.memset(onesb[:], 1.0), writes=["onesb"])
        tr.op("pool", lambda e: e.memset(ones32[:], 1.0), writes=["ones32"])
        tr.op("pool", lambda e: e.memset(epsc[:], EPS), writes=["epsc"])
        s_r = [tr.slot("ld_r%d" % i) for i in range(4)]
        k = 0
        for key in [kk for kk in rowpos if not (isinstance(kk, tuple) and kk[-1] == "ap")]:
            ap = rowpos[(key, "ap")]
            r0 = rowpos[key]
            n = ap.shape[0]
            tr.dma("sp", s_r[k % 4], lambda e, ap=ap, r0=r0, n=n: e.dma_start(out=R[r0 % 128:(r0 % 128) + n, r0 // 128, :], in_=ap),
                   reads=[], writes=["R"])
            k += 1
        s_b = tr.slot("ld_br")
        tr.dma("sp", s_b, lambda e: e.dma_start(out=brt[:], in_=moe_b_router.rearrange("o e -> e o")), writes=["brt"])
        tr.op("dve", lambda e: e.tensor_copy(out=identb[:], in_=ident), reads=["cst"], writes=["identb"])
        for rt in range(NRT):
            b = rt % 4
            tr.op("pe", lambda e, rt=rt, b=b: e.transpose(bank(b, 128), R[:, rt, :], ident), reads=["R", "cst"], writes=[("ps", b)])
            tr.op("dve", lambda e, rt=rt, b=b: e.tensor_copy(out=PC[:, rt * 128:(rt + 1) * 128], in_=bank(b, 128)),
                  reads=[("ps", b)], writes=["PC"])
        xin = [sb(es, "xin%d" % i, [128, D], F32) for i in range(2)]
        xins = [tr.slot("xin%d" % i) for i in range(2)]
        xo = [sb(es, "xo%d" % i, [128, KD, 128], F32) for i in range(2)]
        xos = [tr.slot("xo%d" % i) for i in range(2)]
        xTv = xT.rearrange("(c p) t -> p c t", p=128)
        nb = 0
        for tt in range(NTT):
            i = tt % 2
            tr.dma("sp", xins[i], lambda e, i=i, tt=tt: e.dma_start(out=xin[i][:], in_=x_d[tt * 128:(tt + 1) * 128, :]), writes=[("xin", i)])
            for c0 in range(0, KD, 4):
                b = nb % 8
                nb += 1
                ncc = min(4, KD - c0)
                for cc in range(ncc):
                    c = c0 + cc
                    tr.op("pe", lambda e, i=i, c=c, b=b, cc=cc: e.transpose(bank(b, 128, cc * 128), xin[i][:, c * 128:(c + 1) * 128], ident),
                          reads=[("xin", i), "cst"], writes=[("ps", b)])
                eng = "act" if (nb % 2 == 0) else "dve"
                if eng == "act":
                    tr.op("act", lambda e, i=i, c0=c0, b=b, ncc=ncc: e.copy(out=xo[i][:, c0:c0 + ncc, :], in_=bank(b, ncc * 128).rearrange("p (c t) -> p c t", t=128)),
                          reads=[("ps", b)], writes=[("xo", i)])
                else:
                    tr.op("dve", lambda e, i=i, c0=c0, b=b, ncc=ncc: e.tensor_copy(out=xo[i][:, c0:c0 + ncc, :], in_=bank(b, ncc * 128).rearrange("p (c t) -> p c t", t=128)),
                          reads=[("ps", b)], writes=[("xo", i)])
            for c0 in range(0, KD, 8):
                ncc = min(8, KD - c0)
                tr.dma("sp", xos[i], lambda e, i=i, tt=tt, c0=c0, ncc=ncc: e.dma_start(out=xTv[:, c0:c0 + ncc, tt * 128:(tt + 1) * 128], in_=xo[i][:, c0:c0 + ncc, :]),
                       reads=[("xo", i)], writes=["xT"])
        csb = sb(es, "csb", [128, KD], BF16)
        tr.op("act", lambda e: e.activation(out=csb[:], in_=pcol("c", 0, KD), func=AF.Silu), reads=["PC"], writes=["csb"])
        G = 4
        ngr = MODW // G
        WA = [sb(es, "WA%d" % i, [128, KD, G * 128], BF16) for i in range(2)]
        WAs = [tr.slot("wa%d" % i) for i in range(2)]
        modps = bank(7, MODW) if MODW <= 512 else None
        assert MODW <= 512
        for g in range(ngr):
            i = g % 2
            hk = max(KD // 2, 1)
            for h0 in range(0, KD, hk):
                src = w_ada[h0 * 128:(h0 + hk) * 128, g * G * 128:(g + 1) * G * 128].rearrange("(kc p) n -> p kc n", p=128)
                tr.dma("pool", WAs[i], lambda e, src=src, i=i, h0=h0, hk=hk: e.dma_start(out=WA[i][:, h0:h0 + hk, :], in_=src),
                       writes=[("WA", i)])
            for q in range(G):
                j = g * G + q
                for kc in range(KD):
                    tr.op("pe", lambda e, i=i, q=q, kc=kc, j=j: e.matmul(PSA[:, 7 * 512 + j: 7 * 512 + j + 1], WA[i][:, kc, q * 128:(q + 1) * 128],
                                                                       csb[:, kc:kc + 1], start=(kc == 0), stop=(kc == KD - 1)),
                          reads=[("WA", i), "csb"], writes=[("ps", 7)], inc=(kc == KD - 1))
        for l in range(2):
            for m in range(6):
                tr.op("dve", lambda e, l=l, m=m: e.tensor_tensor(out=Ml[l][:, m * KD:(m + 1) * KD], in0=pcol(("b_ada", m), 0, KD),
                                                               in1=pcol(("ada", l, m), 0, KD), op=ALU.add),
                      reads=["PC"], writes=[("Ml", l)])
            tr.op("dve", lambda e, l=l: e.tensor_tensor(out=Ml[l][:], in0=Ml[l][:], in1=modps, op=ALU.add),
                  reads=[("ps", 7), ("Ml", l)], writes=[("Ml", l)])
            tr.op("dve", lambda e, l=l: e.scalar_tensor_tensor(out=S1[l][:], in0=Ml[l][:, 1 * KD:2 * KD], scalar=1.0, in1=pcol(("nmw", l), 0, KD),
                                                              op0=ALU.add, op1=ALU.mult), reads=[("Ml", l), "PC"], writes=[("S1", l)])
            tr.op("dve", lambda e, l=l: e.scalar_tensor_tensor(out=S2[l][:], in0=Ml[l][:, 4 * KD:5 * KD], scalar=1.0, in1=pcol(("nfw", l), 0, KD),
                                                              op0=ALU.add, op1=ALU.mult), reads=[("Ml", l), "PC"], writes=[("S2", l)])
            tr.op("dve", lambda e, l=l: e.tensor_scalar(out=wqs[l][:], in0=pcol(("qnw", l)), scalar1=float(128 ** -0.5), scalar2=None, op0=ALU.mult),
                  reads=["PC"], writes=[("wqs", l)])
        tr.op("pool", lambda e: e.memset(lbv[0][:], 0.0), writes=[("lbv", 0)])
        tr.op("dve", lambda e: e.tensor_tensor(out=lbv[1][:], in0=pcol(("lb", 1), 0, AH), in1=pcol(("lb", 0), 0, AH), op=ALU.subtract),
              reads=["PC"], writes=[("lbv", 1)])
        tr.op("act", lambda e: e.activation(out=lbv[1][:], in_=lbv[1][:], func=AF.Sigmoid), reads=[("lbv", 1)], writes=[("lbv", 1)])
        for l in range(2):
            tr.op("dve", lambda e, l=l: e.tensor_scalar(out=oml[l][:], in0=lbv[l][:], scalar1=-1.0, scalar2=1.0, op0=ALU.mult, op1=ALU.add),
                  reads=[("lbv", l)], writes=[("oml", l)])
            tr.op("dve", lambda e, l=l: e.tensor_scalar(out=fml[l][:], in0=lbv[l][:], scalar1=-1.0, scalar2=F_MIN, op0=ALU.mult, op1=ALU.add),
                  reads=[("lbv", l)], writes=[("fml", l)])

        rb = sb(es, "rb", [BH, 257], F32)
        zrow = sb(es, "zrow", [BH, 768], F32)
        zst = [sb(es, "zst%d" % i, [128, 768], F32) for i in range(2)]
        zss = [tr.slot("zs%d" % i) for i in range(2)]
        sel = sb(es, "sel", [BH, BH, 128], F32)
        s_rb = tr.slot("ld_rb")
        tr.dma("sp", s_rb, lambda e: e.dma_start(out=rb[:], in_=rel_bias[:, :]), writes=["rb"])
        tr.op("dve", lambda e: e.tensor_copy(out=zrow[:, 0:129], in_=rb[:, 128:257]), reads=["rb"], writes=["zrow"])
        tr.op("dve", lambda e: e.tensor_copy(out=zrow[:, 129:641], in_=rb[:, 256:257].to_broadcast([BH, 512])), reads=["rb"], writes=["zrow"])
        tr.op("dve", lambda e: e.tensor_copy(out=zrow[:, 641:768], in_=rb[:, 1:128]), reads=["rb"], writes=["zrow"])
        tr.op("dve", lambda e: e.tensor_copy(out=sel[:], in_=cst[0:BH, 0:BH].unsqueeze(2).to_broadcast([BH, BH, 128])), reads=["cst"], writes=["sel"])
        for h in range(BH):
            i = h % 2
            for s in range(2):
                tr.op("pe", lambda e, h=h, s=s: e.matmul(bank(s, 384), sel[:, h, :], zrow[:, s * 384:(s + 1) * 384], start=True, stop=True),
                      reads=["sel", "zrow"], writes=[("ps", s)])
            for s in range(2):
                tr.op("dve", lambda e, i=i, s=s: e.tensor_copy(out=zst[i][:, s * 384:(s + 1) * 384], in_=bank(s, 384)),
                      reads=[("ps", s)], writes=[("zst", i)])
            tr.dma("sp", zss[i], lambda e, h=h, i=i: e.dma_start(out=Zd[h].rearrange("(r m) -> r m", m=768), in_=zst[i][:]),
                   reads=[("zst", i)], writes=["Zd"])

        tr.barrier()

    def norm_phase(es, hT, Sv, shv, router_l=None):
        HT = 1024
        xc = [sb(es, "xc%d" % i, [128, HT], F32) for i in range(3)]
        xcs = [tr.slot("xc%d" % i) for i in range(3)]
        sq = [sb(es, "sq%d" % i, [128, HT], BF16) for i in range(2)]
        rs = sb(es, "rs", [128, HT], F32)
        tmp = [sb(es, "ntmp%d" % i, [128, HT], F32) for i in range(2)]
        wr = None
        if router_l is not None:
            wr = sb(es, "wrt", [128, KD, NE], F32)
            s_wr = tr.slot("ld_wr")
            tr.dma("sp", s_wr, lambda e: e.dma_start(out=wr[:], in_=moe_w_router[router_l].rearrange("(c p) e -> p c e", p=128)), writes=["wrt"])
        n = 0
        for hf in range(NH):
            t0 = hf * HT
            for c in range(KD):
                i = n % 3
                n += 1
                tr.dma("sp", xcs[i], lambda e, i=i, c=c, t0=t0: e.dma_start(out=xc[i][:], in_=xT[c * 128:(c + 1) * 128, t0:t0 + HT]), reads=["xT"], writes=[("xc", i)])
                tr.op("act", lambda e, i=i, c=c: e.activation(out=sq[c % 2][:], in_=xc[i][:], func=AF.Square), reads=[("xc", i)], writes=[("sq", c % 2)])
                for tg in range(2):
                    tr.op("pe", lambda e, c=c, tg=tg: e.matmul(bank(tg), onesb[:], sq[c % 2][:, tg * 512:(tg + 1) * 512], start=(c == 0), stop=(c == KD - 1)),
                          reads=[("sq", c % 2), "onesb"], writes=[("ps", tg)], inc=(tg == 1))
            for tg in range(2):
                tr.op("act", lambda e, tg=tg: e.activation(out=rs[:, tg * 512:(tg + 1) * 512], in_=bank(tg), func=AF.Sqrt, bias=epsc[:], scale=1.0 / D),
                      reads=[("ps", tg), "epsc"], writes=["rs"])
            tr.op("dve", lambda e: e.reciprocal(out=rs[:], in_=rs[:]), reads=["rs"], writes=["rs"])
            for c in range(KD):
                i = n % 3
                n += 1
                j = c % 2
                tr.dma("sp", xcs[i], lambda e, i=i, c=c, t0=t0: e.dma_start(out=xc[i][:], in_=xT[c * 128:(c + 1) * 128, t0:t0 + HT]), reads=["xT"], writes=[("xc", i)])
                tr.op("dve", lambda e, i=i, c=c, j=j: e.scalar_tensor_tensor(out=tmp[j][:], in0=xc[i][:], scalar=Sv[:, c:c + 1], in1=rs[:], op0=ALU.mult, op1=ALU.mult),
                      reads=[("xc", i), "rs", "Sv"], writes=[("ntmp", j)])
                if router_l is None:
                    tr.op("act", lambda e, c=c, j=j, t0=t0: e.activation(out=hT[:, c, t0:t0 + HT], in_=tmp[j][:], func=AF.Identity, bias=shv[:, c:c + 1], scale=1.0),
                          reads=[("ntmp", j)], writes=["hT"])
                else:
                    tr.op("act", lambda e, c=c, j=j: e.activation(out=tmp[j][:], in_=tmp[j][:], func=AF.Identity, bias=shv[:, c:c + 1], scale=1.0),
                          reads=[("ntmp", j)], writes=[("ntmp", j)])
                    tr.op("pool", lambda e, c=c, j=j, t0=t0: e.tensor_copy(out=hT[:, c, t0:t0 + HT], in_=tmp[j][:]), reads=[("ntmp", j)], writes=["hT"])
                    for tg in range(2):
                        bb_ = 4 + hf * 2 + tg
                        tr.op("pe", lambda e, c=c, j=j, tg=tg, bb_=bb_: e.matmul(PSA[0:NE, bb_ * 512:(bb_ + 1) * 512], wr[:, c, :], tmp[j][:, tg * 512:(tg + 1) * 512],
                                                                     start=(c == 0), stop=(c == KD - 1)),
                              reads=[("ntmp", j), "wrt"], writes=[("ps", bb_)], inc=(tg == 1))

    def make_copy_evac(es, dst_rows):
        stg = [sb(es, "cstg%d" % i, [128, 1024], F32) for i in range(4)]
        stgs = [tr.slot("cstg%d" % i) for i in range(4)]
        st = {"n": 0}

        def evac(infos, hf, pss):
            (ps, pskey) = pss[0]
            n = st["n"]
            st["n"] += 1
            i = n % 4
            if n % 2 == 0:
                tr.op("act", lambda e: e.copy(out=stg[i][:], in_=ps), reads=pskey, writes=[("cstg", i)])
            else:
                tr.op("dve", lambda e: e.tensor_copy(out=stg[i][:], in_=ps), reads=pskey, writes=[("cstg", i)])
            r0 = dst_rows(infos[0])
            tr.dma("sp", stgs[i], lambda e: e.dma_start(out=projT[r0:r0 + 128, hf * 1024:(hf + 1) * 1024], in_=stg[i][:]),
                   reads=[("cstg", i)], writes=["projT"])
        return evac

    def make_rmw_evac(es, gate_col, units):
        xs = [sb(es, "xs%d" % i, [128, 1024], F32) for i in range(4)]
        xss = [tr.slot("xs%d" % i) for i in range(4)]
        st = {"n": 0, "ld": 0}

        def load(n):
            if n >= len(units):
                return
            (j, hf) = units[n]
            i = n % 4
            tr.dma("sp", xss[i], lambda e: e.dma_start(out=xs[i][:], in_=xT[j * 128:(j + 1) * 128, hf * 1024:(hf + 1) * 1024]),
                   reads=[("xT", j, hf)], writes=[("xs", i)])
        load(0)
        load(1)

        def evac(infos, hf, pss):
            (ps, pskey) = pss[0]
            n = st["n"]
            st["n"] += 1
            i = n % 4
            (j, hf2) = units[n]
            assert hf2 == hf and j == infos[0]
            load(n + 2)
            tr.op("dve", lambda e: e.scalar_tensor_tensor(out=xs[i][:], in0=ps, scalar=gate_col(j), in1=xs[i][:], op0=ALU.mult, op1=ALU.add),
                  reads=pskey + [("xs", i)], writes=[("xs", i)])
            tr.dma("sp", xss[i], lambda e: e.dma_start(out=xT[j * 128:(j + 1) * 128, hf * 1024:(hf + 1) * 1024], in_=xs[i][:]),
                   reads=[("xs", i)], writes=[("xT", j, hf)])
        return evac

    def make_glu_evac(es, row0_of, comb=None):
        sg = [sb(es, "sg%d" % i, [128, 1024], F32) for i in range(2)]
        ast = [sb(es, "ast%d" % i, [128, 1024], BF16) for i in range(3)]
        asts = [tr.slot("ast%d" % i) for i in range(3)]
        st = {"n": 0}

        def evac(infos, hf, pss):
            (pg, pgk), (pu, puk) = pss
            n = st["n"]
            st["n"] += 1
            i = n % 2
            a = n % 3
            tr.op("act", lambda e: e.activation(out=sg[i][:], in_=pg, func=AF.Silu), reads=pgk, writes=[("sg", i)])
            if comb is None:
                tr.op("dve", lambda e: e.tensor_tensor(out=ast[a][:], in0=sg[i][:], in1=pu, op=ALU.mult), reads=puk + [("sg", i)], writes=[("ast", a)])
            else:
                cb, cbkey = comb()
                tr.op("dve", lambda e: e.tensor_tensor(out=sg[i][:], in0=sg[i][:], in1=pu, op=ALU.mult), reads=puk + [("sg", i)], writes=[("sg", i)])
                tr.op("dve", lambda e: e.tensor_tensor(out=ast[a][:], in0=sg[i][:], in1=cb[:, hf * 1024:(hf + 1) * 1024], op=ALU.mult),
                      reads=[("sg", i), cbkey], writes=[("ast", a)])
            r0 = row0_of(infos[0])
            tr.dma("sp", asts[a], lambda e: e.dma_start(out=actT[r0:r0 + 128, hf * 1024:(hf + 1) * 1024], in_=ast[a][:]),
                   reads=[("ast", a)], writes=["actT"])
        return evac

    def load_A(AT, atkey, row0, nkc, src, slots):
        for kc in range(nkc):
            tr.dma("sp", slots[kc % len(slots)], lambda e, kc=kc: e.dma_start(out=AT[:, kc, :], in_=src[row0 + kc * 128: row0 + (kc + 1) * 128, :]),
                   reads=["srcA"], writes=[atkey])

    def mixers(l):
        with ExitStack() as es:
            NIN = 6
            inb = [sb(es, "inb%d" % i, [128, T], F32) for i in range(NIN)]
            inbs = [tr.slot("inb%d" % i) for i in range(NIN)]
            ist = {"n": 0}

            def load_rows(r0):
                i = ist["n"] % NIN
                ist["n"] += 1
                tr.dma("sp", inbs[i], lambda e: e.dma_start(out=inb[i][:], in_=projT[r0:r0 + 128, :]), reads=["projT"], writes=[("inb", i)])
                return i

            t1 = sb(es, "t1", [128, T], F32)
            t2 = sb(es, "t2", [128, T], F32)
            t3 = sb(es, "t3", [128, T], F32)
            q1 = sb(es, "q1", [128, T], BF16)
            k1 = sb(es, "k1", [128, T], BF16)
            vb = sb(es, "vb", [128, T], BF16)
            vT = sb(es, "vT", [128, NTT, 128], BF16)
            yst = [sb(es, "yst%d" % i, [128, T], BF16) for i in range(2)]
            esA = ExitStack()
            bb = sb(esA, "bb", [128, T], F32)
            kh = sb(esA, "kh", [128, T], BF16)
            khT = sb(esA, "khT", [128, NTT, 128], BF16)
            Sall = sb(esA, "Sall", [128, NCH, 128], F32)
            dc = sb(esA, "dc", [128, NCH], F32)
            am = [sb(esA, "am%d" % i, [128, 128], BF16) for i in range(2)]
            ysts = [tr.slot("yst%d" % i) for i in range(2)]
            yn = {"n": 0}
            NB = T // 512

            def store_y(tile_idx, i):
                tr.dma("sp", ysts[i], lambda e: e.dma_start(out=yT_d[tile_idx * 128:(tile_idx + 1) * 128, :], in_=yst[i][:]),
                       reads=[("yst", i)], writes=["yT"])

            def transpose_bf(src, srckey, dst, dstkey, banks):
                for t0 in range(0, NTT, 8):
                    b = banks[(t0 // 8) % len(banks)]
                    nn = min(8, NTT - t0)
                    for tt in range(nn):
                        tr.op("pe", lambda e, t0=t0, tt=tt, b=b: e.transpose(PSB[:, b * 1024 + tt * 128: b * 1024 + (tt + 1) * 128],
                                                                          src[:, (t0 + tt) * 128:(t0 + tt + 1) * 128], identb[:]),
                              reads=[srckey, "identb"], writes=[("ps", b)])
                    tr.op("act", lambda e, t0=t0, nn=nn, b=b: e.copy(out=dst[:, t0:t0 + nn, :],
                                                                  in_=PSB[:, b * 1024: b * 1024 + nn * 128].rearrange("p (t f) -> p t f", f=128)),
                          reads=[("ps", b)], writes=[dstkey])

            def headnorm_rstd(src_ap_fn, srckeys, dstt, dstkey, banks, sqt, sqkey):
                for tg in range(NB):
                    tr.op("act", lambda e, tg=tg: e.activation(out=sqt[:, tg * 512:(tg + 1) * 512], in_=src_ap_fn(tg), func=AF.Square),
                          reads=srckeys(tg), writes=[sqkey])
                for tg in range(NB):
                    b = banks[tg]
                    tr.op("pe", lambda e, tg=tg, b=b: e.matmul(bank(b), ones32[:], sqt[:, tg * 512:(tg + 1) * 512], start=True, stop=True),
                          reads=[sqkey, "ones32"], writes=[("ps", b)])
                    tr.op("act", lambda e, tg=tg, b=b: e.activation(out=dstt[:, tg * 512:(tg + 1) * 512], in_=bank(b), func=AF.Sqrt, bias=epsc[:], scale=1.0 / 128),
                          reads=[("ps", b), "epsc"], writes=[dstkey])
                tr.op("dve", lambda e: e.reciprocal(out=dstt[:], in_=dstt[:]), reads=[dstkey], writes=[dstkey])

            for h in (range(AH) if "A" in groups else []):
                iq = load_rows(0 * AW + h * 128)
                iz = load_rows(1 * AW + h * 128)
                iv = load_rows(2 * AW + h * 128)
                ig = load_rows(3 * AW + h * 128)
                qk, zk, vk, gk = ("inb", iq), ("inb", iz), ("inb", iv), ("inb", ig)
                q32, z32, v32, g32 = inb[iq], inb[iz], inb[iv], inb[ig]
                lbc, omc, fmc = lbv[l][:, h:h + 1], oml[l][:, h:h + 1], fml[l][:, h:h + 1]
                tr.op("act", lambda e: e.activation(out=t1[:], in_=z32[:], func=AF.Sigmoid), reads=[zk], writes=["t1"])
                tr.op("act", lambda e: e.activation(out=t2[:], in_=z32[:], func=AF.Sigmoid, scale=-1.0), reads=[zk], writes=["t2"])
                tr.op("dve", lambda e: e.tensor_scalar(out=t1[:], in0=t1[:], scalar1=omc, scalar2=fmc, op0=ALU.mult, op1=ALU.max),
                      reads=["t1", ("oml", l), ("fml", l)], writes=["t1"])
                tr.op("act", lambda e: e.activation(out=t1[:], in_=t1[:], func=AF.Ln, bias=lbc, scale=1.0), reads=["t1", ("lbv", l)], writes=["t1"])
                tr.op("dve", lambda e: e.tensor_tensor_scan(out=bb[:], data0=rmask, data1=t1[:], initial=0.0, op0=ALU.mult, op1=ALU.add),
                      reads=["t1", "cst"], writes=["bb"])
                tr.op("act", lambda e: e.activation(out=t2[:], in_=t2[:], func=AF.Identity, scale=omc), reads=["t2", ("oml", l)], writes=["t2"])
                b3 = bb[:, :].rearrange("p (c s) -> p c s", s=CH)
                tr.op("pool", lambda e: e.tensor_tensor(out=t1[:, :].rearrange("p (c s) -> p c s", s=CH), in0=b3,
                                                      in1=b3[:, :, 31:32].to_broadcast([128, NCH, CH]), op=ALU.subtract), reads=["bb"], writes=["t1"])
                tr.op("act", lambda e: e.activation(out=t3[:], in_=t1[:], func=AF.Exp), reads=["t1"], writes=["t3"])
                tr.op("dve", lambda e: e.scalar_tensor_tensor(out=q1[:], in0=q32[:], scalar=float(2.0 ** -30), in1=t3[:], op0=ALU.mult, op1=ALU.mult), reads=[qk, "t3"], writes=["q1"])
                tr.op("act", lambda e: e.activation(out=t3[:], in_=t1[:], func=AF.Exp, scale=-1.0), reads=["t1"], writes=["t3"])
                tr.op("dve", lambda e: e.scalar_tensor_tensor(out=k1[:], in0=t2[:], scalar=float(2.0 ** -30), in1=t3[:], op0=ALU.mult, op1=ALU.mult), reads=["t2", "t3"], writes=["k1"])
                tr.op("pool", lambda e: e.tensor_tensor(out=t1[:, :].rearrange("p (c s) -> p c s", s=CH), in0=b3[:, :, CH - 1:CH].to_broadcast([128, NCH, CH]),
                                                      in1=b3, op=ALU.subtract), reads=["bb"], writes=["t1"])
                tr.op("act", lambda e: e.activation(out=t1[:], in_=t1[:], func=AF.Exp), reads=["t1"], writes=["t1"])
                tr.op("pool", lambda e: e.tensor_tensor(out=kh[:], in0=t2[:], in1=t1[:], op=ALU.mult), reads=["t2", "t1"], writes=["kh"])
                tr.op("act", lambda e: e.activation(out=dc[:], in_=b3[:, :, CH - 1], func=AF.Exp), reads=["bb"], writes=["dc"])
                tr.op("act", lambda e: e.activation(out=t3[:], in_=bb[:], func=AF.Exp), reads=["bb"], writes=["t3"])
                tr.op("pool", lambda e: e.tensor_tensor(out=q32[:], in0=q32[:], in1=t3[:], op=ALU.mult), reads=[qk, "t3", "q1"], writes=[qk])
                tr.op("act", lambda e: e.copy(out=vb[:], in_=v32[:]), reads=[vk], writes=["vb"])
                transpose_bf(kh, "kh", khT, "khT", [6, 7])
                transpose_bf(vb, "vb", vT, "vT", [6, 7])
                for c in range(NCH):
                    tb, hh = c // 2, c % 2
                    ub = c % 2
                    tr.op("pe", lambda e, tb=tb, hh=hh, ub=ub: e.matmul(bank(ub, 128), khT[hh * 64:(hh + 1) * 64, tb, :], vT[hh * 64:(hh + 1) * 64, tb, :],
                                                                      start=True, stop=True), reads=["khT", "vT"], writes=[("ps", ub)])
                    if c == 0:
                        tr.op("dve", lambda e, ub=ub: e.tensor_copy(out=Sall[:, 0, :], in_=bank(ub, 128)), reads=[("ps", ub)], writes=["Sall"])
                    else:
                        tr.op("dve", lambda e, c=c, ub=ub: e.scalar_tensor_tensor(out=Sall[:, c, :], in0=Sall[:, c - 1, :], scalar=dc[:, c:c + 1],
                                                                                in1=bank(ub, 128), op0=ALU.mult, op1=ALU.add),
                              reads=[("ps", ub), "Sall", "dc"], writes=["Sall"])
                for tb in range(NTT):
                    ab = tb % 2
                    tr.op("pe", lambda e, tb=tb, ab=ab: e.matmul(bank(6 + ab, 128), k1[:, tb * 128:(tb + 1) * 128], q1[:, tb * 128:(tb + 1) * 128],
                                                               start=True, stop=True), reads=["k1", "q1"], writes=[("ps", 6 + ab)])
                    tr.op("dve", lambda e, ab=ab: e.tensor_tensor(out=am[ab][:], in0=bank(6 + ab, 128), in1=cmask, op=ALU.mult),
                          reads=[("ps", 6 + ab), "cst"], writes=[("am", ab)])
                    ob = 2 + (tb * 128) // 512
                    oo = (tb * 128) % 512
                    tr.op("pe", lambda e, tb=tb, ab=ab, ob=ob, oo=oo: e.matmul(bank(ob, 128, oo), vT[:, tb, :], am[ab][:], start=True, stop=False, skip_group_check=True),
                          reads=["vT", ("am", ab)], writes=[("ps", ob)])
                    for hh in range(2):
                        c = 2 * tb + hh
                        if c == 0:
                            continue
                        tr.op("pe", lambda e, c=c, ob=ob, oo=oo, hh=hh: e.matmul(bank(ob, 64, oo + hh * 64), Sall[:, c - 1, :], q32[:, c * CH:(c + 1) * CH],
                                                                              start=False, stop=True, skip_group_check=True),
                              reads=["Sall", qk], writes=[("ps", ob)])
                headnorm_rstd(lambda tg: bank(2 + tg), lambda tg: [("ps", 2 + tg)], t1, "t1", [6, 7, 6, 7][:NB], t3, "t3")
                tr.op("act", lambda e: e.activation(out=g32[:], in_=g32[:], func=AF.Silu), reads=[gk], writes=[gk])
                for tg in range(NB):
                    tr.op("dve", lambda e, tg=tg: e.tensor_tensor(out=t3[:, tg * 512:(tg + 1) * 512], in0=bank(2 + tg), in1=t1[:, tg * 512:(tg + 1) * 512], op=ALU.mult),
                          reads=[("ps", 2 + tg), "t1", "t3"], writes=["t3"])
                yi = yn["n"] % 2
                yn["n"] += 1
                tr.op("dve", lambda e, yi=yi: e.scalar_tensor_tensor(out=yst[yi][:], in0=t3[:], scalar=pcol(("hnw", l)), in1=g32[:], op0=ALU.mult, op1=ALU.mult),
                      reads=["t3", gk, "PC"], writes=[("yst", yi)])
                store_y(h, yi)

            tr.barrier()
            esA.close()
            esB = ExitStack()
            qn = q1
            kn = k1
            Eb = sb(esB, "Eb", [128, NTT, 640], BF16)
            bias = [sb(esB, "bias%d" % i, [128, 640], F32) for i in range(2)]
            biass = [tr.slot("bias%d" % i) for i in range(2)]
            sc = [sb(esB, "sc%d" % i, [128, 640], F32) for i in range(2)]
            a3 = 4 * AW
            for h in (range(BH) if "B" in groups else []):
                iq = load_rows(a3 + 0 * BW + h * 128)
                ik = load_rows(a3 + 1 * BW + h * 128)
                iv = load_rows(a3 + 2 * BW + h * 128)
                qk, kk, vk = ("inb", iq), ("inb", ik), ("inb", iv)
                q32, k32, v32 = inb[iq], inb[ik], inb[iv]
                bi = h % 2
                zsrc = bass.AP(tensor=Zd.tensor, offset=Zd[h].offset, ap=[[767, 128], [1, 640]])
                tr.dma("sp", biass[bi], lambda e, bi=bi, zsrc=zsrc: e.dma_start(out=bias[bi][:], in_=zsrc), reads=["Zd"], writes=[("bias", bi)])
                tr.op("pool", lambda e, bi=bi: e.memset(bias[bi][64:128, 0:64], NEG), reads=[], writes=[("bias", bi)])
                tr.op("pool", lambda e, bi=bi: e.memset(bias[bi][0:64, 576:640], NEG), reads=[], writes=[("bias", bi)])
                headnorm_rstd(lambda tg: q32[:, tg * 512:(tg + 1) * 512], lambda tg: [qk], t1, "t1", [0, 1, 2, 3], t3, "t3")
                tr.op("dve", lambda e: e.scalar_tensor_tensor(out=qn[:], in0=q32[:], scalar=wqs[l][:, 0:1], in1=t1[:], op0=ALU.mult, op1=ALU.mult),
                      reads=[qk, "t1", ("wqs", l)], writes=["q1"])
                headnorm_rstd(lambda tg: k32[:, tg * 512:(tg + 1) * 512], lambda tg: [kk], t2, "t2", [4, 5, 6, 7], t3, "t3")
                tr.op("dve", lambda e: e.scalar_tensor_tensor(out=kn[:], in0=k32[:], scalar=pcol(("knw", l)), in1=t2[:], op0=ALU.mult, op1=ALU.mult),
                      reads=[kk, "t2", "PC"], writes=["k1"])
                tr.op("act", lambda e: e.copy(out=vb[:], in_=v32[:]), reads=[vk], writes=["vb"])
                transpose_bf(vb, "vb", vT, "vT", [6, 7])
                for kb in range(NTT):
                    nq = min(640, T - kb * 128)
                    sb_ = kb % 2
                    pb = 2 * (kb % 2)
                    parts = [(0, min(512, nq))] + ([(512, nq - 512)] if nq > 512 else [])
                    for pi, (o, n) in enumerate(parts):
                        tr.op("pe", lambda e, kb=kb, o=o, n=n, pb=pb, pi=pi: e.matmul(bank(pb + pi, n), kn[:, kb * 128:(kb + 1) * 128],
                                                                                   qn[:, kb * 128 + o: kb * 128 + o + n], start=True, stop=True),
                              reads=["k1", "q1"], writes=[("ps", pb + pi)])
                        tr.op("dve", lambda e, o=o, n=n, pb=pb, pi=pi, sb_=sb_, bi=bi: e.tensor_tensor(out=sc[sb_][:, o:o + n], in0=bank(pb + pi, n),
                                                                                                    in1=bias[bi][:, o:o + n], op=ALU.add),
                              reads=[("ps", pb + pi), ("bias", bi)], writes=[("sc", sb_)])
                    tr.op("act", lambda e, kb=kb, nq=nq, sb_=sb_: e.activation(out=Eb[:, kb, 0:nq], in_=sc[sb_][:, 0:nq], func=AF.Exp),
                          reads=[("sc", sb_)], writes=["Eb"])
                yi = yn["n"] % 2
                yn["n"] += 1
                for g in range(NTT):
                    ob = 4 + (g // 4) % 2
                    db = 6 + (g // 4) % 2
                    oo = (g % 4) * 128
                    kbs = list(range(max(0, g - 4), g + 1))
                    for idx, kb in enumerate(kbs):
                        eo = (g - kb) * 128
                        tr.op("pe", lambda e, g=g, kb=kb, ob=ob, oo=oo, eo=eo, idx=idx, kbs=kbs: e.matmul(bank(ob, 128, oo), vT[:, kb, :], Eb[:, kb, eo:eo + 128],
                                                                                                    start=(idx == 0), stop=(idx == len(kbs) - 1), skip_group_check=True),
                              reads=["vT", "Eb"], writes=[("ps", ob)])
                        tr.op("pe", lambda e, g=g, kb=kb, db=db, oo=oo, eo=eo, idx=idx, kbs=kbs: e.matmul(bank(db, 128, oo), onesb[:], Eb[:, kb, eo:eo + 128],
                                                                                                    start=(idx == 0), stop=(idx == len(kbs) - 1), skip_group_check=True),
                              reads=["onesb", "Eb"], writes=[("ps", db)])
                    if g % 4 == 3:
                        g0 = g - 3
                        tr.op("dve", lambda e, g0=g0, db=db: e.reciprocal(out=t3[:, g0 * 128:(g0 + 4) * 128], in_=bank(db)), reads=[("ps", db)], writes=["t3"])
                        tr.op("dve", lambda e, g0=g0, ob=ob, yi=yi: e.tensor_tensor(out=yst[yi][:, g0 * 128:(g0 + 4) * 128], in0=bank(ob),
                                                                                 in1=t3[:, g0 * 128:(g0 + 4) * 128], op=ALU.mult),
                              reads=[("ps", ob), "t3"], writes=[("yst", yi)])
                store_y(AH + h, yi)

            tr.barrier()
            esB.close()
            PGT = cfg.PGT
            b3_ = a3 + 3 * BW
            mixed = sb(es, "mixed", [128, PGT, T], BF16)
            wp = sb(es, "wp", [128, PGT, cfg.PG], BF16)
            wps = tr.slot("wp")
            for gi in (range(4) if "C" in groups else []):
                w = 2 ** (gi + 1)
                tr.dma("pool", wps, lambda e, gi=gi: e.dma_start(out=wp[:], in_=w_pool[l, gi].rearrange("(kc p) n -> p kc n", p=128)),
                       reads=[], writes=["wp"])
                for kt in range(PGT):
                    ip = load_rows(b3_ + (gi * PGT + kt) * 128)
                    pk = ("inb", ip)
                    p32 = inb[ip]
                    cur, curk = p32, pk
                    bufs = [(t1, "t1"), (t2, "t2")]
                    sh = 1
                    bi_ = 0
                    while sh < w:
                        dst, dstk = bufs[bi_ % 2]
                        bi_ += 1
                        tr.op("pool" if (bi_ % 2 == 0) else "dve", lambda e, dst=dst, cur=cur, sh=sh: e.tensor_tensor(out=dst[:, sh:T], in0=cur[:, sh:T], in1=cur[:, 0:T - sh], op=ALU.add),
                              reads=[curk], writes=[dstk])
                        tr.op("act", lambda e, dst=dst, cur=cur, sh=sh: e.copy(out=dst[:, 0:sh], in_=cur[:, 0:sh]), reads=[curk], writes=[dstk])
                        cur, curk = dst, dstk
                        sh *= 2
                    tr.op("dve", lambda e, cur=cur, kt=kt, w=w: e.scalar_tensor_tensor(out=mixed[:, kt, :], in0=cur[:], scalar=1.0 / w, in1=p32[:], op0=ALU.mult, op1=ALU.subtract),
                          reads=[curk, pk], writes=["mixed"])
                    tr.op("dve", lambda e, cur=cur, w=w: e.tensor_tensor(out=t3[:, 0:w], in0=cur[:, 0:w], in1=invc[:, 0:w], op=ALU.mult), reads=[curk, "cst"], writes=["t3"])
                    tr.op("dve", lambda e, kt=kt, w=w: e.tensor_tensor(out=mixed[:, kt, 0:w], in0=t3[:, 0:w], in1=p32[:, 0:w], op=ALU.subtract),
                          reads=["t3", pk, "mixed"], writes=["mixed"])
                for ot in range(PGT):
                    yi = yn["n"] % 2
                    yn["n"] += 1
                    ti = gi * PGT + ot
                    for tg in range(NB):
                        b = 4 * (ot % 2) + tg % 4
                        for kt in range(PGT):
                            tr.op("pe", lambda e, ot=ot, tg=tg, kt=kt, b=b: e.matmul(bank(b), wp[:, kt, ot * 128:(ot + 1) * 128], mixed[:, kt, tg * 512:(tg + 1) * 512],
                                                                                  start=(kt == 0), stop=(kt == PGT - 1)), reads=["wp", "mixed"], writes=[("ps", b)])
                        tr.op("act", lambda e, tg=tg, b=b, yi=yi, ti=ti: e.activation(out=yst[yi][:, tg * 512:(tg + 1) * 512], in_=bank(b), func=AF.Copy,
                                                                                   scale=pcol(("psc", l), ti)), reads=[("ps", b), "PC"], writes=[("yst", yi)])
                    store_y(2 * AH + ti, yi)
            tr.barrier()

    for l in range(2):
        with ExitStack() as es:
            hT = sb(es, "hT", [128, KD, T], BF16)
            with ExitStack() as es2:
                norm_phase(es2, hT, S1[l], Ml[l][:, 0:KD])
                tr.barrier()
            with ExitStack() as es2:
                tiles = [{"pieces": [(0, KD, w_in[l][:, j * 128:(j + 1) * 128])], "KC": KD, "info": j} for j in range(cfg.IN_COLS // 128)]
                gemm(hT, "hT", tiles, make_copy_evac(es2, lambda j: j * 128))
                tr.barrier()
        if stop_after == ("inproj", l):
            break
        mixers(l)
        if stop_after == ("mix", l):
            break
        with ExitStack() as es:
            AT = sb(es, "AT", [128, 32, T], BF16)
            ats = [tr.slot("atl%d" % i) for i in range(4)]
            load_A(AT, "AT", 0, KD, yT_d, ats)
            tiles = [{"pieces": [(0, KD, w_o[l][:, j * 128:(j + 1) * 128])], "KC": KD, "info": j} for j in range(KD)]
            units = [(j, hf) for j in range(KD) for hf in range(NH)]
            gemm(AT, "AT", tiles, make_rmw_evac(es, lambda j: Ml[l][:, 2 * KD + j:2 * KD + j + 1], units))
            tr.barrier()
        if stop_after == ("oproj", l):
            break
        with ExitStack() as es:
            hT = sb(es, "hT", [128, KD, T], BF16)
            if l % 2 == 0:
                with ExitStack() as es2:
                    norm_phase(es2, hT, S2[l], Ml[l][:, 3 * KD:4 * KD])
                    tr.barrier()
                with ExitStack() as es2:
                    NF = DFF // 128
                    tiles = []
                    for f in range(NF):
                        tiles.append({"pieces": [(0, KD, ffn_w_gate[0][:, f * 128:(f + 1) * 128])], "KC": KD, "info": f})
                        tiles.append({"pieces": [(0, KD, ffn_w_up[0][:, f * 128:(f + 1) * 128])], "KC": KD, "info": f})
                    gemm(hT, "hT", tiles, make_glu_evac(es2, lambda f: f * 128), pair=True)
                    tr.barrier()
                NFT = NF
                wdown = lambda f0, nkc, j: [(0, nkc, ffn_w_down[0][f0 * 128:(f0 + nkc) * 128, j * 128:(j + 1) * 128])]
                blocks = []
                nblk = (NF + 31) // 32
                base, rem = NF // nblk, NF % nblk
                f0 = 0
                for b in range(nblk):
                    n = base + (1 if b < rem else 0)
                    blocks.append((f0, n, f0))
                    f0 += n
            else:
                with ExitStack() as esn:
                    norm_phase(esn, hT, S2[l], Ml[l][:, 3 * KD:4 * KD], router_l=0)
                    tr.barrier()
                with ExitStack() as es2:
                    NB = T // 512
                    lg = sb(es2, "lg", [NE, T], F32)
                    for tg in range(NB):
                        tr.op("act", lambda e, tg=tg: e.activation(out=lg[:, tg * 512:(tg + 1) * 512], in_=PSA[0:NE, (4 + tg) * 512:(5 + tg) * 512],
                                                                 func=AF.Identity, bias=brt[:, 0:1], scale=1.0), reads=[("ps", 4 + tg), "brt"], writes=["lg"])
                    lt = sb(es2, "lt", [128, NTT, NE], F32)
                    for tt in range(NTT):
                        tr.op("pe", lambda e, tt=tt: e.transpose(bank(0, NE, tt * NE), lg[:, tt * 128:(tt + 1) * 128], ident[0:NE, 0:NE]),
                              reads=["lg", "cst"], writes=[("ps", 0)])
                    tr.op("dve", lambda e: e.tensor_copy(out=lt[:], in_=bank(0, NTT * NE).rearrange("p (t e) -> p t e", e=NE)), reads=[("ps", 0)], writes=["lt"])
                    m1 = sb(es2, "m1", [128, NTT], F32)
                    m2 = sb(es2, "m2", [128, NTT], F32)
                    eq1 = sb(es2, "eq1", [128, NTT, NE], F32)
                    eq2 = sb(es2, "eq2", [128, NTT, NE], F32)
                    l2 = sb(es2, "l2", [128, NTT, NE], F32)
                    w1 = sb(es2, "w1", [128, NTT], F32)
                    w2 = sb(es2, "w2", [128, NTT], F32)
                    comb = sb(es2, "comb", [128, NTT, NE], F32)
                    bc = lambda t: t[:, :].unsqueeze(2).to_broadcast([128, NTT, NE])
                    tr.op("dve", lambda e: e.tensor_reduce(out=m1[:], in_=lt[:], axis=AX.X, op=ALU.max), reads=["lt"], writes=["m1"])
                    tr.op("dve", lambda e: e.tensor_tensor(out=eq1[:], in0=lt[:], in1=bc(m1), op=ALU.is_equal), reads=["lt", "m1"], writes=["eq1"])
                    tr.op("dve", lambda e: e.scalar_tensor_tensor(out=l2[:], in0=eq1[:], scalar=-1e30, in1=lt[:], op0=ALU.mult, op1=ALU.add),
                          reads=["eq1", "lt"], writes=["l2"])
                    tr.op("dve", lambda e: e.tensor_reduce(out=m2[:], in_=l2[:], axis=AX.X, op=ALU.max), reads=["l2"], writes=["m2"])
                    tr.op("dve", lambda e: e.tensor_tensor(out=eq2[:], in0=l2[:], in1=bc(m2), op=ALU.is_equal), reads=["l2", "m2"], writes=["eq2"])
                    tr.op("dve", lambda e: e.tensor_tensor(out=m1[:], in0=m1[:], in1=m2[:], op=ALU.subtract), reads=["m1", "m2", "eq1"], writes=["m1"])
                    tr.op("act", lambda e: e.activation(out=w1[:], in_=m1[:], func=AF.Sigmoid), reads=["m1"], writes=["w1"])
                    tr.op("act", lambda e: e.activation(out=w2[:], in_=m1[:], func=AF.Sigmoid, scale=-1.0), reads=["m1"], writes=["w2"])
                    tr.op("dve", lambda e: e.tensor_tensor(out=eq1[:], in0=eq1[:], in1=bc(w1), op=ALU.mult), reads=["eq1", "w1"], writes=["eq1"])
                    tr.op("dve", lambda e: e.tensor_tensor(out=eq2[:], in0=eq2[:], in1=bc(w2), op=ALU.mult), reads=["eq2", "w2"], writes=["eq2"])
                    tr.op("dve", lambda e: e.tensor_tensor(out=comb[:], in0=eq1[:], in1=eq2[:], op=ALU.add), reads=["eq1", "eq2"], writes=["comb"])
                    dg = [sb(es2, "dg%d" % i, [128, NE, 128], F32) for i in range(2)]
                    cbs = [sb(es2, "cbs%d" % i, [128, NE, 128], F32) for i in range(2)]
                    cbss = [tr.slot("cbs%d" % i) for i in range(2)]
                    combv = combT.rearrange("e p t -> p e t")
                    for tt in range(NTT):
                        i = tt % 2
                        tr.op("dve", lambda e, tt=tt, i=i: e.tensor_tensor(out=dg[i][:], in0=ident.unsqueeze(1).to_broadcast([128, NE, 128]),
                                                                        in1=comb[:, tt, :].unsqueeze(2).to_broadcast([128, NE, 128]), op=ALU.mult),
                              reads=["comb", "cst"], writes=[("dg", i)])
                        for s in range(2):
                            b = 2 + 2 * i + s
                            tr.op("pe", lambda e, i=i, s=s, b=b: e.matmul(bank(b), ones32[:], dg[i][:, s * (NE // 2):(s + 1) * (NE // 2), :].rearrange("p e t -> p (e t)"),
                                                                        start=True, stop=True), reads=[("dg", i), "ones32"], writes=[("ps", b)])
                            tr.op("act", lambda e, i=i, s=s, b=b: e.copy(out=cbs[i][:, s * (NE // 2):(s + 1) * (NE // 2), :].rearrange("p e t -> p (e t)"), in_=bank(b)),
                                  reads=[("ps", b)], writes=[("cbs", i)])
                        tr.dma("sp", cbss[i], lambda e, i=i, tt=tt: e.dma_start(out=combv[:, :, tt * 128:(tt + 1) * 128], in_=cbs[i][:]),
                               reads=[("cbs", i)], writes=["combT"])
                    tr.barrier()
                with ExitStack() as es2:
                    NFE = DEXP // 128
                    cb = [sb(es2, "cb%d" % i, [128, T], F32) for i in range(2)]
                    cbsl = [tr.slot("cb%d" % i) for i in range(2)]
                    cur = {"e": -1}

                    def comb_of():
                        return cb[cur["e"] % 2], ("cb", cur["e"] % 2)
                    evac = make_glu_evac(es2, lambda info: info[0] * DEXP + info[1] * 128, comb=comb_of)
                    for e_ in range(NE):
                        cur["e"] = e_
                        tr.dma("sp", cbsl[e_ % 2], lambda e, e_=e_: e.dma_start(out=cb[e_ % 2][:], in_=combT[e_]), reads=["combT"], writes=[("cb", e_ % 2)])
                        tiles = []
                        for f in range(NFE):
                            tiles.append({"pieces": [(0, KD, moe_w_gate[0, e_][:, f * 128:(f + 1) * 128])], "KC": KD, "info": (e_, f)})
                            tiles.append({"pieces": [(0, KD, moe_w_up[0, e_][:, f * 128:(f + 1) * 128])], "KC": KD, "info": (e_, f)})
                        gemm(hT, "hT", tiles, evac, pair=True)
                    tr.barrier()
                blocks = []
                nb_e = (NFE + 31) // 32
                for e_ in range(NE):
                    base, rem = NFE // nb_e, NFE % nb_e
                    f0 = 0
                    for b in range(nb_e):
                        n = base + (1 if b < rem else 0)
                        blocks.append((e_ * NFE + f0, n, (e_, f0)))
                        f0 += n
                wdown = lambda f0, nkc, j: [(0, nkc, moe_w_down[0, f0[0]][f0[1] * 128:(f0[1] + nkc) * 128, j * 128:(j + 1) * 128])]
        if stop_after == ("up", l):
            break
        with ExitStack() as es:
            AT = sb(es, "AT", [128, 32, T], BF16)
            ats = [tr.slot("atl%d" % i) for i in range(4)]
            units = [(j, hf) for _ in blocks for j in range(KD) for hf in range(NH)]
            evac = make_rmw_evac(es, lambda j: Ml[l][:, 5 * KD + j:5 * KD + j + 1], units)
            for (arow, nkc, wkey) in blocks:
                load_A(AT, "AT", arow * 128, nkc, actT, ats)
                tiles = [{"pieces": wdown(wkey, nkc, j), "KC": nkc, "info": j} for j in range(KD)]
                gemm(AT, "AT", tiles, evac)
            tr.barrier()

    with ExitStack() as es:
        xc = [sb(es, "fxc%d" % i, [128, KD, 128], F32) for i in range(2)]
        xcs = [tr.slot("fxc%d" % i) for i in range(2)]
        xo = [sb(es, "fxo%d" % i, [128, D], F32) for i in range(2)]
        xos = [tr.slot("fxo%d" % i) for i in range(2)]
        xTv = xT.rearrange("(c p) t -> p c t", p=128)
        nb = 0
        for tt in range(NTT):
            i = tt % 2
            for c0 in range(0, KD, 8):
                ncc = min(8, KD - c0)
                tr.dma("sp", xcs[i], lambda e, i=i, tt=tt, c0=c0, ncc=ncc: e.dma_start(out=xc[i][:, c0:c0 + ncc, :], in_=xTv[:, c0:c0 + ncc, tt * 128:(tt + 1) * 128]),
                       reads=["xT"], writes=[("fxc", i)])
            for c0 in range(0, KD, 4):
                b = nb % 8
                nb += 1
                ncc = min(4, KD - c0)
                for cc in range(ncc):
                    tr.op("pe", lambda e, i=i, c0=c0, cc=cc, b=b: e.transpose(bank(b, 128, cc * 128), xc[i][:, c0 + cc, :], ident),
                          reads=[("fxc", i), "cst"], writes=[("ps", b)])
                if nb % 2 == 0:
                    tr.op("act", lambda e, i=i, c0=c0, ncc=ncc, b=b: e.copy(out=xo[i][:, c0 * 128:(c0 + ncc) * 128], in_=bank(b, ncc * 128)),
                          reads=[("ps", b)], writes=[("fxo", i)])
                else:
                    tr.op("dve", lambda e, i=i, c0=c0, ncc=ncc, b=b: e.tensor_copy(out=xo[i][:, c0 * 128:(c0 + ncc) * 128], in_=bank(b, ncc * 128)),
                          reads=[("ps", b)], writes=[("fxo", i)])
            tr.dma("sp", xos[i], lambda e, i=i, tt=tt: e.dma_start(out=out_d[tt * 128:(tt + 1) * 128, :], in_=xo[i][:]), reads=[("fxo", i)], writes=["out"])
        tr.barrier()
    ES.close()
    return nc


def make_consts(T):
    c = np.zeros((128, 128 + 128 + T + 16), np.float32)
    c[:, 0:128] = np.eye(128, dtype=np.float32)
    s = np.arange(128)[:, None]
    t = np.arange(128)[None, :]
    c[:, 128:256] = ((s // CH == t // CH) & (s <= t)).astype(np.float32) * np.float32(2.0 ** 60)
    c[:, 256:256 + T] = (np.arange(T) % CH != 0).astype(np.float32)[None, :]
    c[:, 256 + T:256 + T + 16] = (1.0 / (np.arange(16) + 1.0)).astype(np.float32)[None, :]
    return c


def make_in_maps(cfg, inputs, n_cores):
    D, KD, T = cfg.D, cfg.KD, cfg.T
    f = lambda a: np.ascontiguousarray(np.asarray(a, dtype=np.float32))
    shared = {
        "w_ada": f(inputs["w_ada"]),
        "b_ada": f(inputs["b_ada"]).reshape(6 * KD, 128),
        "ada_table": f(inputs["ada_table"]).reshape(2, 6 * KD, 128),
        "norm_mix_w": f(inputs["norm_mix_w"]).reshape(2, KD, 128),
        "w_in": f(inputs["w_in"]),
        "lb_logits": f(inputs["lb_logits"]).reshape(2, cfg.AH, 128),
        "hgrn_norm_w": f(inputs["hgrn_norm_w"]).reshape(2, 1, 128),
        "q_norm_w": f(inputs["q_norm_w"]).reshape(2, 1, 128),
        "k_norm_w": f(inputs["k_norm_w"]).reshape(2, 1, 128),
        "rel_bias": f(inputs["rel_bias"]),
        "w_pool": f(inputs["w_pool"]),
        "pool_scale": f(inputs["pool_scale"]).reshape(2, cfg.CW // 128, 128),
        "w_o": f(inputs["w_o"]),
        "norm_ffn_w": f(inputs["norm_ffn_w"]).reshape(2, KD, 128),
        "ffn_w_gate": f(inputs["ffn_w_gate"]),
        "ffn_w_up": f(inputs["ffn_w_up"]),
        "ffn_w_down": f(inputs["ffn_w_down"]),
        "moe_w_router": f(inputs["moe_w_router"]),
        "moe_b_router": f(inputs["moe_b_router"]),
        "moe_w_gate": f(inputs["moe_w_gate"]),
        "moe_w_up": f(inputs["moe_w_up"]),
        "moe_w_down": f(inputs["moe_w_down"]),
        "consts": make_consts(T),
    }
    x = f(inputs["x"])
    c = f(inputs["c"])
    maps = []
    for b in range(n_cores):
        m = dict(shared)
        m["x"] = x[b]
        m["c"] = c[b].reshape(KD, 128)
        maps.append(m)
    return maps


def kernel(**inputs):
    x = np.asarray(inputs["x"])
    B, S, D = x.shape
    cfg = Cfg(D=D, S=S, n_cores=B)
    nc = build_program(cfg)
    in_maps = make_in_maps(cfg, inputs, B)
    res = run_bass_kernel_spmd(nc, in_maps, core_ids=list(range(B)))
    return np.stack([np.asarray(r["out"], dtype=np.float32) for r in res.results], axis=0)
```

```python
import numpy as np
from contextlib import ExitStack
import concourse.bass as bass
import concourse.mybir as mybir
from concourse.bass_utils import run_bass_kernel_spmd

F32 = mybir.dt.float32
BF16 = mybir.dt.bfloat16
AF = mybir.ActivationFunctionType
ALU = mybir.AluOpType
AX = mybir.AxisListType

EPS = 1e-6
F_MIN = 1e-6
NEG = -30000.0
CH = 64


class Cfg:
    def __init__(self, D=4096, S=2048, n_cores=8):
        self.D = D
        self.T = S
        self.KD = D // 128
        self.AH = (3 * D // 8) // 128
        self.AW = self.AH * 128
        self.BH = self.AH
        self.BW = self.AW
        self.CW = D - self.AW - self.BW
        self.PG = self.CW // 4
        self.PGT = self.PG // 128
        self.IN_COLS = 4 * self.AW + 3 * self.BW + self.CW
        self.DFF = 256 * ((8 * D // 3 + 255) // 256)
        self.NE = 8
        self.DEXP = 5 * D // 4
        self.NMOD = 6
        self.n_cores = n_cores
        self.NH = S // 1024
        self.NTT = S // 128
        self.NCH = S // CH
        assert S % 1024 == 0 and self.PG % 128 == 0


class DSlot:
    _n = 0

    def __init__(self, nc, name):
        DSlot._n += 1
        self.h = nc.alloc_semaphore("%s_%d" % (name, DSlot._n))
        self.v = 0


class TR:
    def __init__(self, nc):
        self.nc = nc
        self.eng = {"pe": nc.tensor, "act": nc.scalar, "dve": nc.vector, "pool": nc.gpsimd, "sp": nc.sync}
        self.sem = {k: nc.alloc_semaphore("cnt_" + k) for k in self.eng}
        self.cnt = {k: 0 for k in self.eng}
        self.lastw = {}
        self.reads = {}
        self.seen = {k: {} for k in self.eng}
        self.slots = []

    def slot(self, name):
        if not hasattr(self, "_slotcache"):
            self._slotcache = {}
        if name in self._slotcache:
            return self._slotcache[name]
        s = DSlot(self.nc, name)
        self.slots.append(s)
        self._slotcache[name] = s
        return s

    def _deps(self, reads, writes):
        deps = []
        for b in reads:
            t = self.lastw.get(b)
            if t is not None:
                deps.append(t)
        for b in writes:
            t = self.lastw.get(b)
            if t is not None:
                deps.append(t)
            r = self.reads.get(b)
            if r:
                deps.extend(r.values())
        return deps

    def _wait(self, e, deps):
        for (s, v, owner) in deps:
            if owner == e and e in ("pe", "sp"):
                continue
            if v <= 0:
                continue
            k = id(s)
            if self.seen[e].get(k, 0) >= v:
                continue
            self.eng[e].wait_ge(s, v)
            self.seen[e][k] = v

    def _record(self, tok, reads, writes):
        for b in writes:
            self.lastw[b] = tok
            self.reads[b] = {}
        for b in reads:
            self.reads.setdefault(b, {})[id(tok[0])] = tok

    def op(self, e, fn, reads=(), writes=(), inc=True):
        self._wait(e, self._deps(reads, writes))
        ins = fn(self.eng[e])
        tok = (self.sem[e], self.cnt[e] + 1, e)
        if inc:
            self.cnt[e] += 1
            ins.then_inc(self.sem[e], 1)
        self._record(tok, reads, writes)
        return ins

    def dma(self, e, slot, fn, reads=(), writes=()):
        deps = self._deps(reads, writes)
        if slot.v > 0:
            deps.append((slot.h, slot.v, None))
        self._wait(e, deps)
        ins = fn(self.eng[e])
        slot.v += 16
        ins.then_inc(slot.h, 16)
        self._record((slot.h, slot.v, None), reads, writes)
        return ins

    def barrier(self):
        toks = [(self.sem[k], self.cnt[k], k) for k in self.eng if self.cnt[k] > 0]
        toks += [(s.h, s.v, None) for s in self.slots if s.v > 0]
        for e in self.eng:
            self._wait(e, [t for t in toks if t[2] != e])
        self.lastw.clear()
        self.reads.clear()


def build_program(cfg, stop_after=None, debug=False, groups="ABC"):
    D, T, KD = cfg.D, cfg.T, cfg.KD
    AH, AW, BH, BW, CW = cfg.AH, cfg.AW, cfg.BH, cfg.BW, cfg.CW
    NH, NTT, NCH = cfg.NH, cfg.NTT, cfg.NCH
    NE, DEXP, DFF = cfg.NE, cfg.DEXP, cfg.DFF
    nc = bass.Bass("TRN2", target_bir_lowering=False)

    def din(name, shape):
        return nc.dram_tensor(name, list(shape), F32, kind="ExternalInput").ap()

    x_d = din("x", [T, D])
    c_d = din("c", [KD, 128])
    w_ada = din("w_ada", [D, 6 * D])
    b_ada = din("b_ada", [6 * KD, 128])
    ada_table = din("ada_table", [2, 6 * KD, 128])
    norm_mix_w = din("norm_mix_w", [2, KD, 128])
    w_in = din("w_in", [2, D, cfg.IN_COLS])
    lb_logits = din("lb_logits", [2, AH, 128])
    hgrn_norm_w = din("hgrn_norm_w", [2, 1, 128])
    q_norm_w = din("q_norm_w", [2, 1, 128])
    k_norm_w = din("k_norm_w", [2, 1, 128])
    rel_bias = din("rel_bias", [BH, 257])
    w_pool = din("w_pool", [2, 4, cfg.PG, cfg.PG])
    pool_scale = din("pool_scale", [2, CW // 128, 128])
    w_o = din("w_o", [2, D, D])
    norm_ffn_w = din("norm_ffn_w", [2, KD, 128])
    ffn_w_gate = din("ffn_w_gate", [1, D, DFF])
    ffn_w_up = din("ffn_w_up", [1, D, DFF])
    ffn_w_down = din("ffn_w_down", [1, DFF, D])
    moe_w_router = din("moe_w_router", [1, D, NE])
    moe_b_router = din("moe_b_router", [1, NE])
    moe_w_gate = din("moe_w_gate", [1, NE, D, DEXP])
    moe_w_up = din("moe_w_up", [1, NE, D, DEXP])
    moe_w_down = din("moe_w_down", [1, NE, DEXP, D])
    NCONST = 128 + 128 + T + 16
    consts_d = din("consts", [128, NCONST])
    out_d = nc.dram_tensor("out", [T, D], F32, kind="ExternalOutput").ap()

    def dscr(name, shape, dt):
        return nc.dram_tensor(name, list(shape), dt, kind=("ExternalOutput" if debug else "Internal")).ap()

    xT = dscr("xT", [D, T], F32)
    projT = dscr("projT", [cfg.IN_COLS, T], F32)
    yT_d = dscr("yT", [D, T], BF16)
    FTOT = max(DFF, NE * DEXP)
    actT = dscr("actT", [FTOT, T], BF16)
    combT = dscr("combT", [NE, 128, T], F32)
    Zd = dscr("Zd", [BH, 128 * 768], F32)

    tr = TR(nc)
    ES = ExitStack()

    sbn = {"n": 0}

    def sb(es, name, shape, dt):
        sbn["n"] += 1
        return es.enter_context(nc.sbuf_tensor("%s_%d" % (name, sbn["n"]), list(shape), dt))

    PSA = ES.enter_context(nc.psum_tensor("PSA", [128, 4096], F32))

    def bank(b, n=512, off=0):
        return PSA[:, b * 512 + off: b * 512 + off + n]

    def unitps(u):
        return PSA[:, u * 1024:(u + 1) * 1024]

    PSB = PSA[:, :].bitcast(BF16)

    cst = sb(ES, "cst", [128, NCONST], F32)
    ident = cst[:, 0:128]
    cmask = cst[:, 128:256]
    rmask = cst[:, 256:256 + T]
    invc = cst[:, 256 + T:256 + T + 16]
    identb = sb(ES, "identb", [128, 128], BF16)
    onesb = sb(ES, "onesb", [128, 128], BF16)
    ones32 = sb(ES, "ones32", [128, 128], F32)
    epsc = sb(ES, "epsc", [128, 1], F32)

    rows = []
    rowpos = {}

    def addrows(key, ap, n):
        start = len(rows)
        if (start % 128) + n > 128:
            start = (start // 128 + 1) * 128
            while len(rows) < start:
                rows.append(None)
        rowpos[key] = start
        for i in range(n):
            rows.append((key, i))
        rowpos[(key, "ap")] = ap

    addrows("c", c_d, KD)
    for m in range(6):
        addrows(("b_ada", m), b_ada[m * KD:(m + 1) * KD, :], KD)
    for l in range(2):
        for m in range(6):
            addrows(("ada", l, m), ada_table[l, m * KD:(m + 1) * KD, :], KD)
        addrows(("nmw", l), norm_mix_w[l], KD)
        addrows(("nfw", l), norm_ffn_w[l], KD)
        addrows(("lb", l), lb_logits[l], AH)
        addrows(("psc", l), pool_scale[l], CW // 128)
        addrows(("hnw", l), hgrn_norm_w[l], 1)
        addrows(("qnw", l), q_norm_w[l], 1)
        addrows(("knw", l), k_norm_w[l], 1)
    NRT = (len(rows) + 127) // 128
    PC = sb(ES, "PC", [128, NRT * 128], F32)

    def pcol(key, i=0, n=1):
        r = rowpos[key] + i
        return PC[:, r:r + n]

    MODW = 6 * KD
    Ml = [sb(ES, "Ml%d" % l, [128, MODW], F32) for l in range(2)]
    S1 = [sb(ES, "S1_%d" % l, [128, KD], F32) for l in range(2)]
    S2 = [sb(ES, "S2_%d" % l, [128, KD], F32) for l in range(2)]
    lbv = [sb(ES, "lbv%d" % l, [128, AH], F32) for l in range(2)]
    oml = [sb(ES, "oml%d" % l, [128, AH], F32) for l in range(2)]
    fml = [sb(ES, "fml%d" % l, [128, AH], F32) for l in range(2)]
    wqs = [sb(ES, "wqs%d" % l, [128, 1], F32) for l in range(2)]
    brt = sb(ES, "brt", [NE, 1], F32)

    NW = 4
    PF = 2
    WR = [sb(ES, "WR%d" % i, [128, 32, 128], BF16) for i in range(NW)]
    WRs = [tr.slot("wr%d" % i) for i in range(NW)]
    wstate = {"n": 0}

    def wload(pieces):
        i = wstate["n"] % NW
        wstate["n"] += 1
        for (kc0, nkc, ap) in pieces:
            src = ap.rearrange("(kc p) n -> p kc n", p=128)
            tr.dma("pool", WRs[i], lambda e, src=src, i=i, kc0=kc0, nkc=nkc: e.dma_start(out=WR[i][:, kc0:kc0 + nkc, :], in_=src),
                   writes=[("WR", i)])
        return i

    ustate = {"n": 0}

    def gemm(AT, atkey, tiles, evac, pair=False):
        nt = len(tiles)
        loaded = {}
        for i in range(min(PF, nt)):
            loaded[i] = wload(tiles[i]["pieces"])
        step = 2 if pair else 1
        for j0 in range(0, nt, step):
            for hf in range(NH):
                pss = []
                for j in range(j0, j0 + step):
                    if hf == 0 and j + PF < nt:
                        loaded[j + PF] = wload(tiles[j + PF]["pieces"])
                    wi = loaded[j]
                    KC = tiles[j]["KC"]
                    u = ustate["n"] % 4
                    ustate["n"] += 1
                    pskey = [("ps", 2 * u), ("ps", 2 * u + 1)]
                    for kc in range(KC):
                        for s in range(2):
                            last = (kc == KC - 1 and s == 1)
                            tr.op("pe", lambda e, u=u, s=s, wi=wi, kc=kc, hf=hf, KC=KC: e.matmul(
                                PSA[:, u * 1024 + s * 512: u * 1024 + (s + 1) * 512],
                                WR[wi][:, kc, :], AT[:, kc, hf * 1024 + s * 512: hf * 1024 + (s + 1) * 512],
                                start=(kc == 0), stop=(kc == KC - 1)),
                                reads=[("WR", wi), atkey], writes=[pskey[s]], inc=last)
                    pss.append((unitps(u), pskey))
                evac([tiles[j]["info"] for j in range(j0, j0 + step)], hf, pss)

    with ExitStack() as es:
        s_c = tr.slot("ld_c")
        tr.dma("sp", s_c, lambda e: e.dma_start(out=cst[:], in_=consts_d[:, :]), writes=["cst"])
        R = sb(es, "R", [128, NRT, 128], F32)
        tr.op("pool", lambda e: e.memset(R[:], 0.0), writes=["R"])
        tr.op("pool", lambda e: e.memset(onesb[:], 1.0), writes=["onesb"])
        tr.op("pool", lambda e: e.memset(ones32[:], 1.0), writes=["ones32"])
        tr.op("pool", lambda e: e.memset(epsc[:], EPS), writes=["epsc"])
        s_r = [tr.slot("ld_r%d" % i) for i in range(4)]
        k = 0
        for key in [kk for kk in rowpos if not (isinstance(kk, tuple) and kk[-1] == "ap")]:
            ap = rowpos[(key, "ap")]
            r0 = rowpos[key]
            n = ap.shape[0]
            tr.dma("sp", s_r[k % 4], lambda e, ap=ap, r0=r0, n=n: e.dma_start(out=R[r0 % 128:(r0 % 128) + n, r0 // 128, :], in_=ap),
                   reads=[], writes=["R"])
            k += 1
        s_b = tr.slot("ld_br")
        tr.dma("sp", s_b, lambda e: e.dma_start(out=brt[:], in_=moe_b_router.rearrange("o e -> e o")), writes=["brt"])
        tr.op("dve", lambda e: e.tensor_copy(out=identb[:], in_=ident), reads=["cst"], writes=["identb"])
        for rt in range(NRT):
            b = rt % 4
            tr.op("pe", lambda e, rt=rt, b=b: e.transpose(bank(b, 128), R[:, rt, :], ident), reads=["R", "cst"], writes=[("ps", b)])
            tr.op("dve", lambda e, rt=rt, b=b: e.tensor_copy(out=PC[:, rt * 128:(rt + 1) * 128], in_=bank(b, 128)),
                  reads=[("ps", b)], writes=["PC"])
        xin = [sb(es, "xin%d" % i, [128, D], F32) for i in range(2)]
        xins = [tr.slot("xin%d" % i) for i in range(2)]
        xo = [sb(es, "xo%d" % i, [128, KD, 128], F32) for i in range(2)]
        xos = [tr.slot("xo%d" % i) for i in range(2)]
        xTv = xT.rearrange("(c p) t -> p c t", p=128)
        nb = 0
        for tt in range(NTT):
            i = tt % 2
            tr.dma("sp", xins[i], lambda e, i=i, tt=tt: e.dma_start(out=xin[i][:], in_=x_d[tt * 128:(tt + 1) * 128, :]), writes=[("xin", i)])
            for c0 in range(0, KD, 4):
                b = nb % 8
                nb += 1
                ncc = min(4, KD - c0)
                for cc in range(ncc):
                    c = c0 + cc
                    tr.op("pe", lambda e, i=i, c=c, b=b, cc=cc: e.transpose(bank(b, 128, cc * 128), xin[i][:, c * 128:(c + 1) * 128], ident),
                          reads=[("xin", i), "cst"], writes=[("ps", b)])
                eng = "act" if (nb % 2 == 0) else "dve"
                if eng == "act":
                    tr.op("act", lambda e, i=i, c0=c0, b=b, ncc=ncc: e.copy(out=xo[i][:, c0:c0 + ncc, :], in_=bank(b, ncc * 128).rearrange("p (c t) -> p c t", t=128)),
                          reads=[("ps", b)], writes=[("xo", i)])
                else:
                    tr.op("dve", lambda e, i=i, c0=c0, b=b, ncc=ncc: e.tensor_copy(out=xo[i][:, c0:c0 + ncc, :], in_=bank(b, ncc * 128).rearrange("p (c t) -> p c t", t=128)),
                          reads=[("ps", b)], writes=[("xo", i)])
            for c0 in range(0, KD, 8):
                ncc = min(8, KD - c0)
                tr.dma("sp", xos[i], lambda e, i=i, tt=tt, c0=c0, ncc=ncc: e.dma_start(out=xTv[:, c0:c0 + ncc, tt * 128:(tt + 1) * 128], in_=xo[i][:, c0:c0 + ncc, :]),
                       reads=[("xo", i)], writes=["xT"])
        csb = sb(es, "csb", [128, KD], BF16)
        tr.op("act", lambda e: e.activation(out=csb[:], in_=pcol("c", 0, KD), func=AF.Silu), reads=["PC"], writes=["csb"])
        G = 4
        ngr = MODW // G
        WA = [sb(es, "WA%d" % i, [128, KD, G * 128], BF16) for i in range(2)]
        WAs = [tr.slot("wa%d" % i) for i in range(2)]
        modps = bank(7, MODW) if MODW <= 512 else None
        assert MODW <= 512
        for g in range(ngr):
            i = g % 2
            hk = max(KD // 2, 1)
            for h0 in range(0, KD, hk):
                src = w_ada[h0 * 128:(h0 + hk) * 128, g * G * 128:(g + 1) * G * 128].rearrange("(kc p) n -> p kc n", p=128)
                tr.dma("pool", WAs[i], lambda e, src=src, i=i, h0=h0, hk=hk: e.dma_start(out=WA[i][:, h0:h0 + hk, :], in_=src),
                       writes=[("WA", i)])
            for q in range(G):
                j = g * G + q
                for kc in range(KD):
                    tr.op("pe", lambda e, i=i, q=q, kc=kc, j=j: e.matmul(PSA[:, 7 * 512 + j: 7 * 512 + j + 1], WA[i][:, kc, q * 128:(q + 1) * 128],
                                                                       csb[:, kc:kc + 1], start=(kc == 0), stop=(kc == KD - 1)),
                          reads=[("WA", i), "csb"], writes=[("ps", 7)], inc=(kc == KD - 1))
        for l in range(2):
            for m in range(6):
                tr.op("dve", lambda e, l=l, m=m: e.tensor_tensor(out=Ml[l][:, m * KD:(m + 1) * KD], in0=pcol(("b_ada", m), 0, KD),
                                                               in1=pcol(("ada", l, m), 0, KD), op=ALU.add),
                      reads=["PC"], writes=[("Ml", l)])
            tr.op("dve", lambda e, l=l: e.tensor_tensor(out=Ml[l][:], in0=Ml[l][:], in1=modps, op=ALU.add),
                  reads=[("ps", 7), ("Ml", l)], writes=[("Ml", l)])
            tr.op("dve", lambda e, l=l: e.scalar_tensor_tensor(out=S1[l][:], in0=Ml[l][:, 1 * KD:2 * KD], scalar=1.0, in1=pcol(("nmw", l), 0, KD),
                                                              op0=ALU.add, op1=ALU.mult), reads=[("Ml", l), "PC"], writes=[("S1", l)])
            tr.op("dve", lambda e, l=l: e.scalar_tensor_tensor(out=S2[l][:], in0=Ml[l][:, 4 * KD:5 * KD], scalar=1.0, in1=pcol(("nfw", l), 0, KD),
                                                              op0=ALU.add, op1=ALU.mult), reads=[("Ml", l), "PC"], writes=[("S2", l)])
            tr.op("dve", lambda e, l=l: e.tensor_scalar(out=wqs[l][:], in0=pcol(("qnw", l)), scalar1=float(128 ** -0.5), scalar2=None, op0=ALU.mult),
                  reads=["PC"], writes=[("wqs", l)])
        tr.op("pool", lambda e: e.memset(lbv[0][:], 0.0), writes=[("lbv", 0)])
        tr.op("dve", lambda e: e.tensor_tensor(out=lbv[1][:], in0=pcol(("lb", 1), 0, AH), in1=pcol(("lb", 0), 0, AH), op=ALU.subtract),
              reads=["PC"], writes=[("lbv", 1)])
        tr.op("act", lambda e: e.activation(out=lbv[1][:], in_=lbv[1][:], func=AF.Sigmoid), reads=[("lbv", 1)], writes=[("lbv", 1)])
        for l in range(2):
            tr.op("dve", lambda e, l=l: e.tensor_scalar(out=oml[l][:], in0=lbv[l][:], scalar1=-1.0, scalar2=1.0, op0=ALU.mult, op1=ALU.add),
                  reads=[("lbv", l)], writes=[("oml", l)])
            tr.op("dve", lambda e, l=l: e.tensor_scalar(out=fml[l][:], in0=lbv[l][:], scalar1=-1.0, scalar2=F_MIN, op0=ALU.mult, op1=ALU.add),
                  reads=[("lbv", l)], writes=[("fml", l)])

        rb = sb(es, "rb", [BH, 257], F32)
        zrow = sb(es, "zrow", [BH, 768], F32)
        zst = [sb(es, "zst%d" % i, [128, 768], F32) for i in range(2)]
        zss = [tr.slot("zs%d" % i) for i in range(2)]
        sel = sb(es, "sel", [BH, BH, 128], F32)
        s_rb = tr.slot("ld_rb")
        tr.dma("sp", s_rb, lambda e: e.dma_start(out=rb[:], in_=rel_bias[:, :]), writes=["rb"])
        tr.op("dve", lambda e: e.tensor_copy(out=zrow[:, 0:129], in_=rb[:, 128:257]), reads=["rb"], writes=["zrow"])
        tr.op("dve", lambda e: e.tensor_copy(out=zrow[:, 129:641], in_=rb[:, 256:257].to_broadcast([BH, 512])), reads=["rb"], writes=["zrow"])
        tr.op("dve", lambda e: e.tensor_copy(out=zrow[:, 641:768], in_=rb[:, 1:128]), reads=["rb"], writes=["zrow"])
        tr.op("dve", lambda e: e.tensor_copy(out=sel[:], in_=cst[0:BH, 0:BH].unsqueeze(2).to_broadcast([BH, BH, 128])), reads=["cst"], writes=["sel"])
        for h in range(BH):
            i = h % 2
            for s in range(2):
                tr.op("pe", lambda e, h=h, s=s: e.matmul(bank(s, 384), sel[:, h, :], zrow[:, s * 384:(s + 1) * 384], start=True, stop=True),
                      reads=["sel", "zrow"], writes=[("ps", s)])
            for s in range(2):
                tr.op("dve", lambda e, i=i, s=s: e.tensor_copy(out=zst[i][:, s * 384:(s + 1) * 384], in_=bank(s, 384)),
                      reads=[("ps", s)], writes=[("zst", i)])
            tr.dma("sp", zss[i], lambda e, h=h, i=i: e.dma_start(out=Zd[h].rearrange("(r m) -> r m", m=768), in_=zst[i][:]),
                   reads=[("zst", i)], writes=["Zd"])

        tr.barrier()

    def norm_phase(es, hT, Sv, shv, router_l=None):
        HT = 1024
        xc = [sb(es, "xc%d" % i, [128, HT], F32) for i in range(3)]
        xcs = [tr.slot("xc%d" % i) for i in range(3)]
        sq = [sb(es, "sq%d" % i, [128, HT], BF16) for i in range(2)]
        rs = sb(es, "rs", [128, HT], F32)
        tmp = [sb(es, "ntmp%d" % i, [128, HT], F32) for i in range(2)]
        wr = None
        if router_l is not None:
            wr = sb(es, "wrt", [128, KD, NE], F32)
            s_wr = tr.slot("ld_wr")
            tr.dma("sp", s_wr, lambda e: e.dma_start(out=wr[:], in_=moe_w_router[router_l].rearrange("(c p) e -> p c e", p=128)), writes=["wrt"])
        n = 0
        for hf in range(NH):
            t0 = hf * HT
            for c in range(KD):
                i = n % 3
                n += 1
                tr.dma("sp", xcs[i], lambda e, i=i, c=c, t0=t0: e.dma_start(out=xc[i][:], in_=xT[c * 128:(c + 1) * 128, t0:t0 + HT]), reads=["xT"], writes=[("xc", i)])
                tr.op("act", lambda e, i=i, c=c: e.activation(out=sq[c % 2][:], in_=xc[i][:], func=AF.Square), reads=[("xc", i)], writes=[("sq", c % 2)])
                for tg in range(2):
                    tr.op("pe", lambda e, c=c, tg=tg: e.matmul(bank(tg), onesb[:], sq[c % 2][:, tg * 512:(tg + 1) * 512], start=(c == 0), stop=(c == KD - 1)),
                          reads=[("sq", c % 2), "onesb"], writes=[("ps", tg)], inc=(tg == 1))
            for tg in range(2):
                tr.op("act", lambda e, tg=tg: e.activation(out=rs[:, tg * 512:(tg + 1) * 512], in_=bank(tg), func=AF.Ln, bias=epsc[:], scale=1.0 / D),
                      reads=[("ps", tg), "epsc"], writes=["rs"])
            tr.op("act", lambda e: e.activation(out=rs[:], in_=rs[:], func=AF.Exp, scale=-0.5), reads=["rs"], writes=["rs"])
            for c in range(KD):
                i = n % 3
                n += 1
                j = c % 2
                tr.dma("sp", xcs[i], lambda e, i=i, c=c, t0=t0: e.dma_start(out=xc[i][:], in_=xT[c * 128:(c + 1) * 128, t0:t0 + HT]), reads=["xT"], writes=[("xc", i)])
                tr.op("dve", lambda e, i=i, c=c, j=j: e.scalar_tensor_tensor(out=tmp[j][:], in0=xc[i][:], scalar=Sv[:, c:c + 1], in1=rs[:], op0=ALU.mult, op1=ALU.mult),
                      reads=[("xc", i), "rs", "Sv"], writes=[("ntmp", j)])
                if router_l is None:
                    tr.op("act", lambda e, c=c, j=j, t0=t0: e.activation(out=hT[:, c, t0:t0 + HT], in_=tmp[j][:], func=AF.Identity, bias=shv[:, c:c + 1], scale=1.0),
                          reads=[("ntmp", j)], writes=["hT"])
                else:
                    tr.op("act", lambda e, c=c, j=j: e.activation(out=tmp[j][:], in_=tmp[j][:], func=AF.Identity, bias=shv[:, c:c + 1], scale=1.0),
                          reads=[("ntmp", j)], writes=[("ntmp", j)])
                    tr.op("pool", lambda e, c=c, j=j, t0=t0: e.tensor_copy(out=hT[:, c, t0:t0 + HT], in_=tmp[j][:]), reads=[("ntmp", j)], writes=["hT"])
                    for tg in range(2):
                        bb_ = 4 + hf * 2 + tg
                        tr.op("pe", lambda e, c=c, j=j, tg=tg, bb_=bb_: e.matmul(PSA[0:NE, bb_ * 512:(bb_ + 1) * 512], wr[:, c, :], tmp[j][:, tg * 512:(tg + 1) * 512],
                                                                     start=(c == 0), stop=(c == KD - 1)),
                              reads=[("ntmp", j), "wrt"], writes=[("ps", bb_)], inc=(tg == 1))

    def make_copy_evac(es, dst_rows):
        stg = [sb(es, "cstg%d" % i, [128, 1024], F32) for i in range(4)]
        stgs = [tr.slot("cstg%d" % i) for i in range(4)]
        st = {"n": 0}

        def evac(infos, hf, pss):
            (ps, pskey) = pss[0]
            n = st["n"]
            st["n"] += 1
            i = n % 4
            if n % 2 == 0:
                tr.op("act", lambda e: e.copy(out=stg[i][:], in_=ps), reads=pskey, writes=[("cstg", i)])
            else:
                tr.op("dve", lambda e: e.tensor_copy(out=stg[i][:], in_=ps), reads=pskey, writes=[("cstg", i)])
            r0 = dst_rows(infos[0])
            tr.dma("sp", stgs[i], lambda e: e.dma_start(out=projT[r0:r0 + 128, hf * 1024:(hf + 1) * 1024], in_=stg[i][:]),
                   reads=[("cstg", i)], writes=["projT"])
        return evac

    def make_rmw_evac(es, gate_col, units):
        xs = [sb(es, "xs%d" % i, [128, 1024], F32) for i in range(4)]
        xss = [tr.slot("xs%d" % i) for i in range(4)]
        st = {"n": 0, "ld": 0}

        def load(n):
            if n >= len(units):
                return
            (j, hf) = units[n]
            i = n % 4
            tr.dma("sp", xss[i], lambda e: e.dma_start(out=xs[i][:], in_=xT[j * 128:(j + 1) * 128, hf * 1024:(hf + 1) * 1024]),
                   reads=[("xT", j, hf)], writes=[("xs", i)])
        load(0)
        load(1)

        def evac(infos, hf, pss):
            (ps, pskey) = pss[0]
            n = st["n"]
            st["n"] += 1
            i = n % 4
            (j, hf2) = units[n]
            assert hf2 == hf and j == infos[0]
            load(n + 2)
            tr.op("dve", lambda e: e.scalar_tensor_tensor(out=xs[i][:], in0=ps, scalar=gate_col(j), in1=xs[i][:], op0=ALU.mult, op1=ALU.add),
                  reads=pskey + [("xs", i)], writes=[("xs", i)])
            tr.dma("sp", xss[i], lambda e: e.dma_start(out=xT[j * 128:(j + 1) * 128, hf * 1024:(hf + 1) * 1024], in_=xs[i][:]),
                   reads=[("xs", i)], writes=[("xT", j, hf)])
        return evac

    def make_glu_evac(es, row0_of, comb=None):
        sg = [sb(es, "sg%d" % i, [128, 1024], F32) for i in range(2)]
        ast = [sb(es, "ast%d" % i, [128, 1024], BF16) for i in range(3)]
        asts = [tr.slot("ast%d" % i) for i in range(3)]
        st = {"n": 0}

        def evac(infos, hf, pss):
            (pg, pgk), (pu, puk) = pss
            n = st["n"]
            st["n"] += 1
            i = n % 2
            a = n % 3
            tr.op("act", lambda e: e.activation(out=sg[i][:], in_=pg, func=AF.Silu), reads=pgk, writes=[("sg", i)])
            if comb is None:
                tr.op("dve", lambda e: e.tensor_tensor(out=ast[a][:], in0=sg[i][:], in1=pu, op=ALU.mult), reads=puk + [("sg", i)], writes=[("ast", a)])
            else:
                cb, cbkey = comb()
                tr.op("dve", lambda e: e.tensor_tensor(out=sg[i][:], in0=sg[i][:], in1=pu, op=ALU.mult), reads=puk + [("sg", i)], writes=[("sg", i)])
                tr.op("dve", lambda e: e.tensor_tensor(out=ast[a][:], in0=sg[i][:], in1=cb[:, hf * 1024:(hf + 1) * 1024], op=ALU.mult),
                      reads=[("sg", i), cbkey], writes=[("ast", a)])
            r0 = row0_of(infos[0])
            tr.dma("sp", asts[a], lambda e: e.dma_start(out=actT[r0:r0 + 128, hf * 1024:(hf + 1) * 1024], in_=ast[a][:]),
                   reads=[("ast", a)], writes=["actT"])
        return evac

    def load_A(AT, atkey, row0, nkc, src, slots):
        for kc in range(nkc):
            tr.dma("sp", slots[kc % len(slots)], lambda e, kc=kc: e.dma_start(out=AT[:, kc, :], in_=src[row0 + kc * 128: row0 + (kc + 1) * 128, :]),
                   reads=["srcA"], writes=[atkey])

    def mixers(l):
        with ExitStack() as es:
            NIN = 6
            inb = [sb(es, "inb%d" % i, [128, T], F32) for i in range(NIN)]
            inbs = [tr.slot("inb%d" % i) for i in range(NIN)]
            ist = {"n": 0}

            def load_rows(r0):
                i = ist["n"] % NIN
                ist["n"] += 1
                tr.dma("sp", inbs[i], lambda e: e.dma_start(out=inb[i][:], in_=projT[r0:r0 + 128, :]), reads=["projT"], writes=[("inb", i)])
                return i

            t1 = sb(es, "t1", [128, T], F32)
            t2 = sb(es, "t2", [128, T], F32)
            t3 = sb(es, "t3", [128, T], F32)
            q1 = sb(es, "q1", [128, T], BF16)
            k1 = sb(es, "k1", [128, T], BF16)
            vb = sb(es, "vb", [128, T], BF16)
            vT = sb(es, "vT", [128, NTT, 128], BF16)
            yst = [sb(es, "yst%d" % i, [128, T], BF16) for i in range(2)]
            esA = ExitStack()
            bb = sb(esA, "bb", [128, T], F32)
            kh = sb(esA, "kh", [128, T], BF16)
            khT = sb(esA, "khT", [128, NTT, 128], BF16)
            Sall = sb(esA, "Sall", [128, NCH, 128], F32)
            dc = sb(esA, "dc", [128, NCH], F32)
            am = [sb(esA, "am%d" % i, [128, 128], BF16) for i in range(2)]
            ysts = [tr.slot("yst%d" % i) for i in range(2)]
            yn = {"n": 0}
            NB = T // 512

            def store_y(tile_idx, i):
                tr.dma("sp", ysts[i], lambda e: e.dma_start(out=yT_d[tile_idx * 128:(tile_idx + 1) * 128, :], in_=yst[i][:]),
                       reads=[("yst", i)], writes=["yT"])

            def transpose_bf(src, srckey, dst, dstkey, banks):
                for t0 in range(0, NTT, 8):
                    b = banks[(t0 // 8) % len(banks)]
                    nn = min(8, NTT - t0)
                    for tt in range(nn):
                        tr.op("pe", lambda e, t0=t0, tt=tt, b=b: e.transpose(PSB[:, b * 1024 + tt * 128: b * 1024 + (tt + 1) * 128],
                                                                          src[:, (t0 + tt) * 128:(t0 + tt + 1) * 128], identb[:]),
                              reads=[srckey, "identb"], writes=[("ps", b)])
                    tr.op("act", lambda e, t0=t0, nn=nn, b=b: e.copy(out=dst[:, t0:t0 + nn, :],
                                                                  in_=PSB[:, b * 1024: b * 1024 + nn * 128].rearrange("p (t f) -> p t f", f=128)),
                          reads=[("ps", b)], writes=[dstkey])

            def headnorm_rstd(src_ap_fn, srckeys, dstt, dstkey, banks, sqt, sqkey):
                for tg in range(NB):
                    tr.op("act", lambda e, tg=tg: e.activation(out=sqt[:, tg * 512:(tg + 1) * 512], in_=src_ap_fn(tg), func=AF.Square),
                          reads=srckeys(tg), writes=[sqkey])
                for tg in range(NB):
                    b = banks[tg]
                    tr.op("pe", lambda e, tg=tg, b=b: e.matmul(bank(b), ones32[:], sqt[:, tg * 512:(tg + 1) * 512], start=True, stop=True),
                          reads=[sqkey, "ones32"], writes=[("ps", b)])
                    tr.op("act", lambda e, tg=tg, b=b: e.activation(out=dstt[:, tg * 512:(tg + 1) * 512], in_=bank(b), func=AF.Ln, bias=epsc[:], scale=1.0 / 128),
                          reads=[("ps", b), "epsc"], writes=[dstkey])
                tr.op("act", lambda e: e.activation(out=dstt[:], in_=dstt[:], func=AF.Exp, scale=-0.5), reads=[dstkey], writes=[dstkey])

            for h in (range(AH) if "A" in groups else []):
                iq = load_rows(0 * AW + h * 128)
                iz = load_rows(1 * AW + h * 128)
                iv = load_rows(2 * AW + h * 128)
                ig = load_rows(3 * AW + h * 128)
                qk, zk, vk, gk = ("inb", iq), ("inb", iz), ("inb", iv), ("inb", ig)
                q32, z32, v32, g32 = inb[iq], inb[iz], inb[iv], inb[ig]
                lbc, omc, fmc = lbv[l][:, h:h + 1], oml[l][:, h:h + 1], fml[l][:, h:h + 1]
                tr.op("act", lambda e: e.activation(out=t1[:], in_=z32[:], func=AF.Sigmoid), reads=[zk], writes=["t1"])
                tr.op("act", lambda e: e.activation(out=t2[:], in_=z32[:], func=AF.Sigmoid, scale=-1.0), reads=[zk], writes=["t2"])
                tr.op("dve", lambda e: e.tensor_scalar(out=t1[:], in0=t1[:], scalar1=omc, scalar2=fmc, op0=ALU.mult, op1=ALU.max),
                      reads=["t1", ("oml", l), ("fml", l)], writes=["t1"])
                tr.op("act", lambda e: e.activation(out=t1[:], in_=t1[:], func=AF.Ln, bias=lbc, scale=1.0), reads=["t1", ("lbv", l)], writes=["t1"])
                tr.op("dve", lambda e: e.tensor_tensor_scan(out=bb[:], data0=rmask, data1=t1[:], initial=0.0, op0=ALU.mult, op1=ALU.add),
                      reads=["t1", "cst"], writes=["bb"])
                tr.op("act", lambda e: e.activation(out=t2[:], in_=t2[:], func=AF.Identity, scale=omc), reads=["t2", ("oml", l)], writes=["t2"])
                b3 = bb[:, :].rearrange("p (c s) -> p c s", s=CH)
                tr.op("pool", lambda e: e.tensor_tensor(out=t1[:, :].rearrange("p (c s) -> p c s", s=CH), in0=b3,
                                                      in1=b3[:, :, 31:32].to_broadcast([128, NCH, CH]), op=ALU.subtract), reads=["bb"], writes=["t1"])
                tr.op("act", lambda e: e.activation(out=t3[:], in_=t1[:], func=AF.Exp), reads=["t1"], writes=["t3"])
                tr.op("dve", lambda e: e.scalar_tensor_tensor(out=q1[:], in0=q32[:], scalar=float(2.0 ** -30), in1=t3[:], op0=ALU.mult, op1=ALU.mult), reads=[qk, "t3"], writes=["q1"])
                tr.op("act", lambda e: e.activation(out=t3[:], in_=t1[:], func=AF.Exp, scale=-1.0), reads=["t1"], writes=["t3"])
                tr.op("dve", lambda e: e.scalar_tensor_tensor(out=k1[:], in0=t2[:], scalar=float(2.0 ** -30), in1=t3[:], op0=ALU.mult, op1=ALU.mult), reads=["t2", "t3"], writes=["k1"])
                tr.op("pool", lambda e: e.tensor_tensor(out=t1[:, :].rearrange("p (c s) -> p c s", s=CH), in0=b3[:, :, CH - 1:CH].to_broadcast([128, NCH, CH]),
                                                      in1=b3, op=ALU.subtract), reads=["bb"], writes=["t1"])
                tr.op("act", lambda e: e.activation(out=t1[:], in_=t1[:], func=AF.Exp), reads=["t1"], writes=["t1"])
                tr.op("pool", lambda e: e.tensor_tensor(out=kh[:], in0=t2[:], in1=t1[:], op=ALU.mult), reads=["t2", "t1"], writes=["kh"])
                tr.op("act", lambda e: e.activation(out=dc[:], in_=b3[:, :, CH - 1], func=AF.Exp), reads=["bb"], writes=["dc"])
                tr.op("act", lambda e: e.activation(out=t3[:], in_=bb[:], func=AF.Exp), reads=["bb"], writes=["t3"])
                tr.op("pool", lambda e: e.tensor_tensor(out=q32[:], in0=q32[:], in1=t3[:], op=ALU.mult), reads=[qk, "t3", "q1"], writes=[qk])
                tr.op("act", lambda e: e.copy(out=vb[:], in_=v32[:]), reads=[vk], writes=["vb"])
                transpose_bf(kh, "kh", khT, "khT", [6, 7])
                transpose_bf(vb, "vb", vT, "vT", [6, 7])
                for c in range(NCH):
                    tb, hh = c // 2, c % 2
                    ub = c % 2
                    tr.op("pe", lambda e, tb=tb, hh=hh, ub=ub: e.matmul(bank(ub, 128), khT[hh * 64:(hh + 1) * 64, tb, :], vT[hh * 64:(hh + 1) * 64, tb, :],
                                                                      start=True, stop=True), reads=["khT", "vT"], writes=[("ps", ub)])
                    if c == 0:
                        tr.op("dve", lambda e, ub=ub: e.tensor_copy(out=Sall[:, 0, :], in_=bank(ub, 128)), reads=[("ps", ub)], writes=["Sall"])
                    else:
                        tr.op("dve", lambda e, c=c, ub=ub: e.scalar_tensor_tensor(out=Sall[:, c, :], in0=Sall[:, c - 1, :], scalar=dc[:, c:c + 1],
                                                                                in1=bank(ub, 128), op0=ALU.mult, op1=ALU.add),
                              reads=[("ps", ub), "Sall", "dc"], writes=["Sall"])
                for tb in range(NTT):
                    ab = tb % 2
                    tr.op("pe", lambda e, tb=tb, ab=ab: e.matmul(bank(6 + ab, 128), k1[:, tb * 128:(tb + 1) * 128], q1[:, tb * 128:(tb + 1) * 128],
                                                               start=True, stop=True), reads=["k1", "q1"], writes=[("ps", 6 + ab)])
                    tr.op("dve", lambda e, ab=ab: e.tensor_tensor(out=am[ab][:], in0=bank(6 + ab, 128), in1=cmask, op=ALU.mult),
                          reads=[("ps", 6 + ab), "cst"], writes=[("am", ab)])
                    ob = 2 + (tb * 128) // 512
                    oo = (tb * 128) % 512
                    tr.op("pe", lambda e, tb=tb, ab=ab, ob=ob, oo=oo: e.matmul(bank(ob, 128, oo), vT[:, tb, :], am[ab][:], start=True, stop=False, skip_group_check=True),
                          reads=["vT", ("am", ab)], writes=[("ps", ob)])
                    for hh in range(2):
                        c = 2 * tb + hh
                        if c == 0:
                            continue
                        tr.op("pe", lambda e, c=c, ob=ob, oo=oo, hh=hh: e.matmul(bank(ob, 64, oo + hh * 64), Sall[:, c - 1, :], q32[:, c * CH:(c + 1) * CH],
                                                                              start=False, stop=True, skip_group_check=True),
                              reads=["Sall", qk], writes=[("ps", ob)])
                headnorm_rstd(lambda tg: bank(2 + tg), lambda tg: [("ps", 2 + tg)], t1, "t1", [6, 7, 6, 7][:NB], t3, "t3")
                tr.op("act", lambda e: e.activation(out=g32[:], in_=g32[:], func=AF.Silu), reads=[gk], writes=[gk])
                for tg in range(NB):
                    tr.op("dve", lambda e, tg=tg: e.tensor_tensor(out=t3[:, tg * 512:(tg + 1) * 512], in0=bank(2 + tg), in1=t1[:, tg * 512:(tg + 1) * 512], op=ALU.mult),
                          reads=[("ps", 2 + tg), "t1", "t3"], writes=["t3"])
                yi = yn["n"] % 2
                yn["n"] += 1
                tr.op("dve", lambda e, yi=yi: e.scalar_tensor_tensor(out=yst[yi][:], in0=t3[:], scalar=pcol(("hnw", l)), in1=g32[:], op0=ALU.mult, op1=ALU.mult),
                      reads=["t3", gk, "PC"], writes=[("yst", yi)])
                store_y(h, yi)

            tr.barrier()
            esA.close()
            esB = ExitStack()
            qn = q1
            kn = k1
            Eb = sb(esB, "Eb", [128, NTT, 640], BF16)
            bias = [sb(esB, "bias%d" % i, [128, 640], F32) for i in range(2)]
            biass = [tr.slot("bias%d" % i) for i in range(2)]
            sc = [sb(esB, "sc%d" % i, [128, 640], F32) for i in range(2)]
            a3 = 4 * AW
            for h in (range(BH) if "B" in groups else []):
                iq = load_rows(a3 + 0 * BW + h * 128)
                ik = load_rows(a3 + 1 * BW + h * 128)
                iv = load_rows(a3 + 2 * BW + h * 128)
                qk, kk, vk = ("inb", iq), ("inb", ik), ("inb", iv)
                q32, k32, v32 = inb[iq], inb[ik], inb[iv]
                bi = h % 2
                zsrc = bass.AP(tensor=Zd.tensor, offset=Zd[h].offset, ap=[[767, 128], [1, 640]])
                tr.dma("sp", biass[bi], lambda e, bi=bi, zsrc=zsrc: e.dma_start(out=bias[bi][:], in_=zsrc), reads=["Zd"], writes=[("bias", bi)])
                tr.op("pool", lambda e, bi=bi: e.memset(bias[bi][64:128, 0:64], NEG), reads=[], writes=[("bias", bi)])
                tr.op("pool", lambda e, bi=bi: e.memset(bias[bi][0:64, 576:640], NEG), reads=[], writes=[("bias", bi)])
                headnorm_rstd(lambda tg: q32[:, tg * 512:(tg + 1) * 512], lambda tg: [qk], t1, "t1", [0, 1, 2, 3], t3, "t3")
                tr.op("dve", lambda e: e.scalar_tensor_tensor(out=qn[:], in0=q32[:], scalar=wqs[l][:, 0:1], in1=t1[:], op0=ALU.mult, op1=ALU.mult),
                      reads=[qk, "t1", ("wqs", l)], writes=["q1"])
                headnorm_rstd(lambda tg: k32[:, tg * 512:(tg + 1) * 512], lambda tg: [kk], t2, "t2", [4, 5, 6, 7], t3, "t3")
                tr.op("dve", lambda e: e.scalar_tensor_tensor(out=kn[:], in0=k32[:], scalar=pcol(("knw", l)), in1=t2[:], op0=ALU.mult, op1=ALU.mult),
                      reads=[kk, "t2", "PC"], writes=["k1"])
                tr.op("act", lambda e: e.copy(out=vb[:], in_=v32[:]), reads=[vk], writes=["vb"])
                transpose_bf(vb, "vb", vT, "vT", [6, 7])
                for kb in range(NTT):
                    nq = min(640, T - kb * 128)
                    sb_ = kb % 2
                    pb = 2 * (kb % 2)
                    parts = [(0, min(512, nq))] + ([(512, nq - 512)] if nq > 512 else [])
                    for pi, (o, n) in enumerate(parts):
                        tr.op("pe", lambda e, kb=kb, o=o, n=n, pb=pb, pi=pi: e.matmul(bank(pb + pi, n), kn[:, kb * 128:(kb + 1) * 128],
                                                                                   qn[:, kb * 128 + o: kb * 128 + o + n], start=True, stop=True),
                              reads=["k1", "q1"], writes=[("ps", pb + pi)])
                        tr.op("dve", lambda e, o=o, n=n, pb=pb, pi=pi, sb_=sb_, bi=bi: e.tensor_tensor(out=sc[sb_][:, o:o + n], in0=bank(pb + pi, n),
                                                                                                    in1=bias[bi][:, o:o + n], op=ALU.add),
                              reads=[("ps", pb + pi), ("bias", bi)], writes=[("sc", sb_)])
                    tr.op("act", lambda e, kb=kb, nq=nq, sb_=sb_: e.activation(out=Eb[:, kb, 0:nq], in_=sc[sb_][:, 0:nq], func=AF.Exp),
                          reads=[("sc", sb_)], writes=["Eb"])
                yi = yn["n"] % 2
                yn["n"] += 1
                for g in range(NTT):
                    ob = 4 + (g // 4) % 2
                    db = 6 + (g // 4) % 2
                    oo = (g % 4) * 128
                    kbs = list(range(max(0, g - 4), g + 1))
                    for idx, kb in enumerate(kbs):
                        eo = (g - kb) * 128
                        tr.op("pe", lambda e, g=g, kb=kb, ob=ob, oo=oo, eo=eo, idx=idx, kbs=kbs: e.matmul(bank(ob, 128, oo), vT[:, kb, :], Eb[:, kb, eo:eo + 128],
                                                                                                    start=(idx == 0), stop=(idx == len(kbs) - 1), skip_group_check=True),
                              reads=["vT", "Eb"], writes=[("ps", ob)])
                        tr.op("pe", lambda e, g=g, kb=kb, db=db, oo=oo, eo=eo, idx=idx, kbs=kbs: e.matmul(bank(db, 128, oo), onesb[:], Eb[:, kb, eo:eo + 128],
                                                                                                    start=(idx == 0), stop=(idx == len(kbs) - 1), skip_group_check=True),
                              reads=["onesb", "Eb"], writes=[("ps", db)])
                    if g % 4 == 3:
                        g0 = g - 3
                        tr.op("act", lambda e, g0=g0, db=db: e.activation(out=t3[:, g0 * 128:(g0 + 4) * 128], in_=bank(db), func=AF.Ln), reads=[("ps", db)], writes=["t3"])
                        tr.op("act", lambda e, g0=g0: e.activation(out=t3[:, g0 * 128:(g0 + 4) * 128], in_=t3[:, g0 * 128:(g0 + 4) * 128], func=AF.Exp, scale=-1.0), reads=["t3"], writes=["t3"])
                        tr.op("dve", lambda e, g0=g0, ob=ob, yi=yi: e.tensor_tensor(out=yst[yi][:, g0 * 128:(g0 + 4) * 128], in0=bank(ob),
                                                                                 in1=t3[:, g0 * 128:(g0 + 4) * 128], op=ALU.mult),
                              reads=[("ps", ob), "t3"], writes=[("yst", yi)])
                store_y(AH + h, yi)

            tr.barrier()
            esB.close()
            PGT = cfg.PGT
            b3_ = a3 + 3 * BW
            mixed = sb(es, "mixed", [128, PGT, T], BF16)
            wp = sb(es, "wp", [128, PGT, cfg.PG], BF16)
            wps = tr.slot("wp")
            for gi in (range(4) if "C" in groups else []):
                w = 2 ** (gi + 1)
                tr.dma("pool", wps, lambda e, gi=gi: e.dma_start(out=wp[:], in_=w_pool[l, gi].rearrange("(kc p) n -> p kc n", p=128)),
                       reads=[], writes=["wp"])
                for kt in range(PGT):
                    ip = load_rows(b3_ + (gi * PGT + kt) * 128)
                    pk = ("inb", ip)
                    p32 = inb[ip]
                    cur, curk = p32, pk
                    bufs = [(t1, "t1"), (t2, "t2")]
                    sh = 1
                    bi_ = 0
                    while sh < w:
                        dst, dstk = bufs[bi_ % 2]
                        bi_ += 1
                        tr.op("pool" if (bi_ % 2 == 0) else "dve", lambda e, dst=dst, cur=cur, sh=sh: e.tensor_tensor(out=dst[:, sh:T], in0=cur[:, sh:T], in1=cur[:, 0:T - sh], op=ALU.add),
                              reads=[curk], writes=[dstk])
                        tr.op("act", lambda e, dst=dst, cur=cur, sh=sh: e.copy(out=dst[:, 0:sh], in_=cur[:, 0:sh]), reads=[curk], writes=[dstk])
                        cur, curk = dst, dstk
                        sh *= 2
                    tr.op("dve", lambda e, cur=cur, kt=kt, w=w: e.scalar_tensor_tensor(out=mixed[:, kt, :], in0=cur[:], scalar=1.0 / w, in1=p32[:], op0=ALU.mult, op1=ALU.subtract),
                          reads=[curk, pk], writes=["mixed"])
                    tr.op("dve", lambda e, cur=cur, w=w: e.tensor_tensor(out=t3[:, 0:w], in0=cur[:, 0:w], in1=invc[:, 0:w], op=ALU.mult), reads=[curk, "cst"], writes=["t3"])
                    tr.op("dve", lambda e, kt=kt, w=w: e.tensor_tensor(out=mixed[:, kt, 0:w], in0=t3[:, 0:w], in1=p32[:, 0:w], op=ALU.subtract),
                          reads=["t3", pk, "mixed"], writes=["mixed"])
                for ot in range(PGT):
                    yi = yn["n"] % 2
                    yn["n"] += 1
                    ti = gi * PGT + ot
                    for tg in range(NB):
                        b = 4 * (ot % 2) + tg % 4
                        for kt in range(PGT):
                            tr.op("pe", lambda e, ot=ot, tg=tg, kt=kt, b=b: e.matmul(bank(b), wp[:, kt, ot * 128:(ot + 1) * 128], mixed[:, kt, tg * 512:(tg + 1) * 512],
                                                                                  start=(kt == 0), stop=(kt == PGT - 1)), reads=["wp", "mixed"], writes=[("ps", b)])
                        tr.op("act", lambda e, tg=tg, b=b, yi=yi, ti=ti: e.activation(out=yst[yi][:, tg * 512:(tg + 1) * 512], in_=bank(b), func=AF.Copy,
                                                                                   scale=pcol(("psc", l), ti)), reads=[("ps", b), "PC"], writes=[("yst", yi)])
                    store_y(2 * AH + ti, yi)
            tr.barrier()

    for l in range(2):
        with ExitStack() as es:
            hT = sb(es, "hT", [128, KD, T], BF16)
            with ExitStack() as es2:
                norm_phase(es2, hT, S1[l], Ml[l][:, 0:KD])
                tr.barrier()
            with ExitStack() as es2:
                tiles = [{"pieces": [(0, KD, w_in[l][:, j * 128:(j + 1) * 128])], "KC": KD, "info": j} for j in range(cfg.IN_COLS // 128)]
                gemm(hT, "hT", tiles, make_copy_evac(es2, lambda j: j * 128))
                tr.barrier()
        if stop_after == ("inproj", l):
            break
        mixers(l)
        if stop_after == ("mix", l):
            break
        with ExitStack() as es:
            AT = sb(es, "AT", [128, 32, T], BF16)
            ats = [tr.slot("atl%d" % i) for i in range(4)]
            load_A(AT, "AT", 0, KD, yT_d, ats)
            tiles = [{"pieces": [(0, KD, w_o[l][:, j * 128:(j + 1) * 128])], "KC": KD, "info": j} for j in range(KD)]
            units = [(j, hf) for j in range(KD) for hf in range(NH)]
            gemm(AT, "AT", tiles, make_rmw_evac(es, lambda j: Ml[l][:, 2 * KD + j:2 * KD + j + 1], units))
            tr.barrier()
        if stop_after == ("oproj", l):
            break
        with ExitStack() as es:
            hT = sb(es, "hT", [128, KD, T], BF16)
            if l % 2 == 0:
                with ExitStack() as es2:
                    norm_phase(es2, hT, S2[l], Ml[l][:, 3 * KD:4 * KD])
                    tr.barrier()
                with ExitStack() as es2:
                    NF = DFF // 128
                    tiles = []
                    for f in range(NF):
                        tiles.append({"pieces": [(0, KD, ffn_w_gate[0][:, f * 128:(f + 1) * 128])], "KC": KD, "info": f})
                        tiles.append({"pieces": [(0, KD, ffn_w_up[0][:, f * 128:(f + 1) * 128])], "KC": KD, "info": f})
                    gemm(hT, "hT", tiles, make_glu_evac(es2, lambda f: f * 128), pair=True)
                    tr.barrier()
                NFT = NF
                wdown = lambda f0, nkc, j: [(0, nkc, ffn_w_down[0][f0 * 128:(f0 + nkc) * 128, j * 128:(j + 1) * 128])]
                blocks = []
                nblk = (NF + 31) // 32
                base, rem = NF // nblk, NF % nblk
                f0 = 0
                for b in range(nblk):
                    n = base + (1 if b < rem else 0)
                    blocks.append((f0, n, f0))
                    f0 += n
            else:
                with ExitStack() as esn:
                    norm_phase(esn, hT, S2[l], Ml[l][:, 3 * KD:4 * KD], router_l=0)
                    tr.barrier()
                with ExitStack() as es2:
                    NB = T // 512
                    lg = sb(es2, "lg", [NE, T], F32)
                    for tg in range(NB):
                        tr.op("act", lambda e, tg=tg: e.activation(out=lg[:, tg * 512:(tg + 1) * 512], in_=PSA[0:NE, (4 + tg) * 512:(5 + tg) * 512],
                                                                 func=AF.Identity, bias=brt[:, 0:1], scale=1.0), reads=[("ps", 4 + tg), "brt"], writes=["lg"])
                    lt = sb(es2, "lt", [128, NTT, NE], F32)
                    for tt in range(NTT):
                        tr.op("pe", lambda e, tt=tt: e.transpose(bank(0, NE, tt * NE), lg[:, tt * 128:(tt + 1) * 128], ident[0:NE, 0:NE]),
                              reads=["lg", "cst"], writes=[("ps", 0)])
                    tr.op("dve", lambda e: e.tensor_copy(out=lt[:], in_=bank(0, NTT * NE).rearrange("p (t e) -> p t e", e=NE)), reads=[("ps", 0)], writes=["lt"])
                    m1 = sb(es2, "m1", [128, NTT], F32)
                    m2 = sb(es2, "m2", [128, NTT], F32)
                    eq1 = sb(es2, "eq1", [128, NTT, NE], F32)
                    eq2 = sb(es2, "eq2", [128, NTT, NE], F32)
                    l2 = sb(es2, "l2", [128, NTT, NE], F32)
                    w1 = sb(es2, "w1", [128, NTT], F32)
                    w2 = sb(es2, "w2", [128, NTT], F32)
                    comb = sb(es2, "comb", [128, NTT, NE], F32)
                    bc = lambda t: t[:, :].unsqueeze(2).to_broadcast([128, NTT, NE])
                    tr.op("dve", lambda e: e.tensor_reduce(out=m1[:], in_=lt[:], axis=AX.X, op=ALU.max), reads=["lt"], writes=["m1"])
                    tr.op("dve", lambda e: e.tensor_tensor(out=eq1[:], in0=lt[:], in1=bc(m1), op=ALU.is_equal), reads=["lt", "m1"], writes=["eq1"])
                    tr.op("dve", lambda e: e.scalar_tensor_tensor(out=l2[:], in0=eq1[:], scalar=-1e30, in1=lt[:], op0=ALU.mult, op1=ALU.add),
                          reads=["eq1", "lt"], writes=["l2"])
                    tr.op("dve", lambda e: e.tensor_reduce(out=m2[:], in_=l2[:], axis=AX.X, op=ALU.max), reads=["l2"], writes=["m2"])
                    tr.op("dve", lambda e: e.tensor_tensor(out=eq2[:], in0=l2[:], in1=bc(m2), op=ALU.is_equal), reads=["l2", "m2"], writes=["eq2"])
                    tr.op("dve", lambda e: e.tensor_tensor(out=m1[:], in0=m1[:], in1=m2[:], op=ALU.subtract), reads=["m1", "m2", "eq1"], writes=["m1"])
                    tr.op("act", lambda e: e.activation(out=w1[:], in_=m1[:], func=AF.Sigmoid), reads=["m1"], writes=["w1"])
                    tr.op("act", lambda e: e.activation(out=w2[:], in_=m1[:], func=AF.Sigmoid, scale=-1.0), reads=["m1"], writes=["w2"])
                    tr.op("dve", lambda e: e.tensor_tensor(out=eq1[:], in0=eq1[:], in1=bc(w1), op=ALU.mult), reads=["eq1", "w1"], writes=["eq1"])
                    tr.op("dve", lambda e: e.tensor_tensor(out=eq2[:], in0=eq2[:], in1=bc(w2), op=ALU.mult), reads=["eq2", "w2"], writes=["eq2"])
                    tr.op("dve", lambda e: e.tensor_tensor(out=comb[:], in0=eq1[:], in1=eq2[:], op=ALU.add), reads=["eq1", "eq2"], writes=["comb"])
                    dg = [sb(es2, "dg%d" % i, [128, NE, 128], F32) for i in range(2)]
                    cbs = [sb(es2, "cbs%d" % i, [128, NE, 128], F32) for i in range(2)]
                    cbss = [tr.slot("cbs%d" % i) for i in range(2)]
                    combv = combT.rearrange("e p t -> p e t")
                    for tt in range(NTT):
                        i = tt % 2
                        tr.op("dve", lambda e, tt=tt, i=i: e.tensor_tensor(out=dg[i][:], in0=ident.unsqueeze(1).to_broadcast([128, NE, 128]),
                                                                        in1=comb[:, tt, :].unsqueeze(2).to_broadcast([128, NE, 128]), op=ALU.mult),
                              reads=["comb", "cst"], writes=[("dg", i)])
                        for s in range(2):
                            b = 2 + 2 * i + s
                            tr.op("pe", lambda e, i=i, s=s, b=b: e.matmul(bank(b), ones32[:], dg[i][:, s * (NE // 2):(s + 1) * (NE // 2), :].rearrange("p e t -> p (e t)"),
                                                                        start=True, stop=True), reads=[("dg", i), "ones32"], writes=[("ps", b)])
                            tr.op("act", lambda e, i=i, s=s, b=b: e.copy(out=cbs[i][:, s * (NE // 2):(s + 1) * (NE // 2), :].rearrange("p e t -> p (e t)"), in_=bank(b)),
                                  reads=[("ps", b)], writes=[("cbs", i)])
                        tr.dma("sp", cbss[i], lambda e, i=i, tt=tt: e.dma_start(out=combv[:, :, tt * 128:(tt + 1) * 128], in_=cbs[i][:]),
                               reads=[("cbs", i)], writes=["combT"])
                    tr.barrier()
                with ExitStack() as es2:
                    NFE = DEXP // 128
                    cb = [sb(es2, "cb%d" % i, [128, T], F32) for i in range(2)]
                    cbsl = [tr.slot("cb%d" % i) for i in range(2)]
                    cur = {"e": -1}

                    def comb_of():
                        return cb[cur["e"] % 2], ("cb", cur["e"] % 2)
                    evac = make_glu_evac(es2, lambda info: info[0] * DEXP + info[1] * 128, comb=comb_of)
                    for e_ in range(NE):
                        cur["e"] = e_
                        tr.dma("sp", cbsl[e_ % 2], lambda e, e_=e_: e.dma_start(out=cb[e_ % 2][:], in_=combT[e_]), reads=["combT"], writes=[("cb", e_ % 2)])
                        tiles = []
                        for f in range(NFE):
                            tiles.append({"pieces": [(0, KD, moe_w_gate[0, e_][:, f * 128:(f + 1) * 128])], "KC": KD, "info": (e_, f)})
                            tiles.append({"pieces": [(0, KD, moe_w_up[0, e_][:, f * 128:(f + 1) * 128])], "KC": KD, "info": (e_, f)})
                        gemm(hT, "hT", tiles, evac, pair=True)
                    tr.barrier()
                blocks = []
                nb_e = (NFE + 31) // 32
                for e_ in range(NE):
                    base, rem = NFE // nb_e, NFE % nb_e
                    f0 = 0
                    for b in range(nb_e):
                        n = base + (1 if b < rem else 0)
                        blocks.append((e_ * NFE + f0, n, (e_, f0)))
                        f0 += n
                wdown = lambda f0, nkc, j: [(0, nkc, moe_w_down[0, f0[0]][f0[1] * 128:(f0[1] + nkc) * 128, j * 128:(j + 1) * 128])]
        if stop_after == ("up", l):
            break
        with ExitStack() as es:
            AT = sb(es, "AT", [128, 32, T], BF16)
            ats = [tr.slot("atl%d" % i) for i in range(4)]
            units = [(j, hf) for _ in blocks for j in range(KD) for hf in range(NH)]
            evac = make_rmw_evac(es, lambda j: Ml[l][:, 5 * KD + j:5 * KD + j + 1], units)
            for (arow, nkc, wkey) in blocks:
                load_A(AT, "AT", arow * 128, nkc, actT, ats)
                tiles = [{"pieces": wdown(wkey, nkc, j), "KC": nkc, "info": j} for j in range(KD)]
                gemm(AT, "AT", tiles, evac)
            tr.barrier()

    with ExitStack() as es:
        xc = [sb(es, "fxc%d" % i, [128, KD, 128], F32) for i in range(2)]
        xcs = [tr.slot("fxc%d" % i) for i in range(2)]
        xo = [sb(es, "fxo%d" % i, [128, D], F32) for i in range(2)]
        xos = [tr.slot("fxo%d" % i) for i in range(2)]
        xTv = xT.rearrange("(c p) t -> p c t", p=128)
        nb = 0
        for tt in range(NTT):
            i = tt % 2
            for c0 in range(0, KD, 8):
                ncc = min(8, KD - c0)
                tr.dma("sp", xcs[i], lambda e, i=i, tt=tt, c0=c0, ncc=ncc: e.dma_start(out=xc[i][:, c0:c0 + ncc, :], in_=xTv[:, c0:c0 + ncc, tt * 128:(tt + 1) * 128]),
                       reads=["xT"], writes=[("fxc", i)])
            for c0 in range(0, KD, 4):
                b = nb % 8
                nb += 1
                ncc = min(4, KD - c0)
                for cc in range(ncc):
                    tr.op("pe", lambda e, i=i, c0=c0, cc=cc, b=b: e.transpose(bank(b, 128, cc * 128), xc[i][:, c0 + cc, :], ident),
                          reads=[("fxc", i), "cst"], writes=[("ps", b)])
                if nb % 2 == 0:
                    tr.op("act", lambda e, i=i, c0=c0, ncc=ncc, b=b: e.copy(out=xo[i][:, c0 * 128:(c0 + ncc) * 128], in_=bank(b, ncc * 128)),
                          reads=[("ps", b)], writes=[("fxo", i)])
                else:
                    tr.op("dve", lambda e, i=i, c0=c0, ncc=ncc, b=b: e.tensor_copy(out=xo[i][:, c0 * 128:(c0 + ncc) * 128], in_=bank(b, ncc * 128)),
                          reads=[("ps", b)], writes=[("fxo", i)])
            tr.dma("sp", xos[i], lambda e, i=i, tt=tt: e.dma_start(out=out_d[tt * 128:(tt + 1) * 128, :], in_=xo[i][:]), reads=[("fxo", i)], writes=["out"])
        tr.barrier()
    ES.close()
    return nc


def make_consts(T):
    c = np.zeros((128, 128 + 128 + T + 16), np.float32)
    c[:, 0:128] = np.eye(128, dtype=np.float32)
    s = np.arange(128)[:, None]
    t = np.arange(128)[None, :]
    c[:, 128:256] = ((s // CH == t // CH) & (s <= t)).astype(np.float32) * np.float32(2.0 ** 60)
    c[:, 256:256 + T] = (np.arange(T) % CH != 0).astype(np.float32)[None, :]
    c[:, 256 + T:256 + T + 16] = (1.0 / (np.arange(16) + 1.0)).astype(np.float32)[None, :]
    return c


def make_in_maps(cfg, inputs, n_cores):
    D, KD, T = cfg.D, cfg.KD, cfg.T
    f = lambda a: np.ascontiguousarray(np.asarray(a, dtype=np.float32))
    shared = {
        "w_ada": f(inputs["w_ada"]),
        "b_ada": f(inputs["b_ada"]).reshape(6 * KD, 128),
        "ada_table": f(inputs["ada_table"]).reshape(2, 6 * KD, 128),
        "norm_mix_w": f(inputs["norm_mix_w"]).reshape(2, KD, 128),
        "w_in": f(inputs["w_in"]),
        "lb_logits": f(inputs["lb_logits"]).reshape(2, cfg.AH, 128),
        "hgrn_norm_w": f(inputs["hgrn_norm_w"]).reshape(2, 1, 128),
        "q_norm_w": f(inputs["q_norm_w"]).reshape(2, 1, 128),
        "k_norm_w": f(inputs["k_norm_w"]).reshape(2, 1, 128),
        "rel_bias": f(inputs["rel_bias"]),
        "w_pool": f(inputs["w_pool"]),
        "pool_scale": f(inputs["pool_scale"]).reshape(2, cfg.CW // 128, 128),
        "w_o": f(inputs["w_o"]),
        "norm_ffn_w": f(inputs["norm_ffn_w"]).reshape(2, KD, 128),
        "ffn_w_gate": f(inputs["ffn_w_gate"]),
        "ffn_w_up": f(inputs["ffn_w_up"]),
        "ffn_w_down": f(inputs["ffn_w_down"]),
        "moe_w_router": f(inputs["moe_w_router"]),
        "moe_b_router": f(inputs["moe_b_router"]),
        "moe_w_gate": f(inputs["moe_w_gate"]),
        "moe_w_up": f(inputs["moe_w_up"]),
        "moe_w_down": f(inputs["moe_w_down"]),
        "consts": make_consts(T),
    }
    x = f(inputs["x"])
    c = f(inputs["c"])
    maps = []
    for b in range(n_cores):
        m = dict(shared)
        m["x"] = x[b]
        m["c"] = c[b].reshape(KD, 128)
        maps.append(m)
    return maps


def kernel(**inputs):
    x = np.asarray(inputs["x"])
    B, S, D = x.shape
    cfg = Cfg(D=D, S=S, n_cores=B)
    nc = build_program(cfg)
    in_maps = make_in_maps(cfg, inputs, B)
    res = run_bass_kernel_spmd(nc, in_maps, core_ids=list(range(B)))
    return np.stack([np.asarray(r["out"], dtype=np.float32) for r in res.results], axis=0)
```
